# Optimizing a Trainium2 kernel written in Bass

```python
import math
import jax, jax.numpy as jnp
from jax import lax
import numpy as np

D_MODEL = 1024
BATCH = 8
SEQ = 8192
DEPTH = 1

N_META = 16
ATTN_HEADS = 4
ATTN_HEAD_DIM = 64
ATTN_VALUE_DIM = 2 * ATTN_HEAD_DIM
ATTN_WIDTH = ATTN_HEADS * ATTN_VALUE_DIM
CONV_WIDTH = D_MODEL - ATTN_WIDTH
CONV_K = 3
MIX_WIDTH = ATTN_WIDTH + CONV_WIDTH
IN_PROJ_WIDTH = 3 * ATTN_WIDTH + 3 * CONV_WIDTH
N_EXPERTS = 16
EC_CAPACITY_FACTOR = 2
D_FF_EXPERT = 2 * D_MODEL
ROPE_THETA = 10000.0
Q_BLOCK = 128
NORM_EPS = 1e-6

kernel_name = "hymba_diffattn_shortconv_ec_moe"


def rms_norm(x, g):
    xf = x.astype(jnp.float32)
    y = xf * lax.rsqrt(jnp.mean(xf * xf, axis=-1, keepdims=True) + NORM_EPS)
    return (y * g.astype(jnp.float32)).astype(x.dtype)


def rope_tables(n):
    d = ATTN_HEAD_DIM
    inv_freq = ROPE_THETA ** (-jnp.arange(0, d, 2, dtype=jnp.float32) / d)
    ang = jnp.arange(n, dtype=jnp.float32)[:, None] * inv_freq[None, :]
    ang = jnp.concatenate([ang, ang], axis=-1)
    return jnp.cos(ang), jnp.sin(ang)


def apply_rope(x, cos, sin):
    half = x.shape[-1] // 2
    x1, x2 = x[..., :half], x[..., half:]
    rot = jnp.concatenate([-x2, x1], axis=-1)
    return (x * cos + rot * sin).astype(x.dtype)


def diff_attention_group(q, k, v, lq1, lk1, lq2, lk2, subln_g, lam_init, cos, sin):
    B, L, _ = q.shape
    Lp = cos.shape[0]
    H, d = ATTN_HEADS, ATTN_HEAD_DIM
    pad = Lp - L
    q = jnp.pad(q.reshape(B, L, H, 2, d), ((0, 0), (0, pad), (0, 0), (0, 0), (0, 0)))
    k = jnp.pad(k.reshape(B, L, H, 2, d), ((0, 0), (0, pad), (0, 0), (0, 0), (0, 0)))
    v = jnp.pad(v.reshape(B, L, H, 2 * d), ((0, 0), (0, pad), (0, 0), (0, 0)))
    q = apply_rope(q.transpose(0, 2, 3, 1, 4), cos, sin)
    k = apply_rope(k.transpose(0, 2, 3, 1, 4), cos, sin)
    v32 = v.transpose(0, 2, 1, 3).astype(jnp.float32)

    lam = (jnp.exp(jnp.sum(lq1.astype(jnp.float32) * lk1.astype(jnp.float32)))
           - jnp.exp(jnp.sum(lq2.astype(jnp.float32) * lk2.astype(jnp.float32)))
           + lam_init)
    key_valid = jnp.arange(Lp) < L
    scale = d ** -0.5
    n_blk = Lp // Q_BLOCK
    qb = jnp.moveaxis(q.reshape(B, H, 2, n_blk, Q_BLOCK, d), 3, 0)

    def one_block(qi):
        s = jnp.einsum('bhiqd,bhikd->bhiqk', qi, k,
                       preferred_element_type=jnp.float32) * scale
        s = jnp.where(key_valid, s, jnp.finfo(jnp.float32).min)
        p = jax.nn.softmax(s, axis=-1)
        a = p[:, :, 0] - lam * p[:, :, 1]
        return jnp.einsum('bhqk,bhkv->bhqv', a, v32)

    o = lax.map(one_block, qb)
    o = o.transpose(1, 0, 3, 2, 4).reshape(B, Lp, H, 2 * d)[:, :L]
    o = rms_norm(o, subln_g) * (1.0 - lam_init)
    return o.reshape(B, L, ATTN_WIDTH).astype(q.dtype)


def centred_depthwise_conv(u, w):
    L = u.shape[1]
    half = CONV_K // 2
    up = jnp.pad(u, ((0, 0), (half, half), (0, 0)))
    return sum(w[j] * up[:, j:j + L] for j in range(CONV_K))


def expert_choice_ffn(h, w_router, w_gate, w_up, w_down):
    B, L, D = h.shape
    cap = EC_CAPACITY_FACTOR * L // N_EXPERTS

    def one_group(hs):
        logits = jnp.einsum('ld,de->le', hs, w_router, preferred_element_type=jnp.float32)
        aff = jax.nn.softmax(logits, axis=-1)
        g, idx = lax.top_k(aff.T, cap)
        xg = hs[idx]
        a = jnp.einsum('ecd,edf->ecf', xg, w_gate)
        u = jnp.einsum('ecd,edf->ecf', xg, w_up)
        y = jnp.einsum('ecf,efd->ecd', jax.nn.silu(a) * u, w_down)
        y = y * g[..., None].astype(y.dtype)
        out = jnp.zeros((L, D), y.dtype).at[idx.reshape(-1)].add(y.reshape(-1, D))
        return out.astype(hs.dtype)

    return lax.map(one_group, h)


def setup_inputs(seed: int = 0) -> dict:
    key = jax.random.key(seed)
    ks = jax.random.split(key, 20)
    f32 = jnp.float32
    nrm = lambda k, shape, s: jax.random.normal(k, shape, f32) * s
    gain = lambda k, shape: 1.0 + 0.02 * jax.random.normal(k, shape, f32)
    return {
        "x": nrm(ks[0], (BATCH, SEQ, D_MODEL), 1.0),
        "meta_tokens": nrm(ks[1], (N_META, D_MODEL), 1.0),
        "mix_norm_g": gain(ks[2], (DEPTH, D_MODEL)),
        "w_in": nrm(ks[3], (DEPTH, D_MODEL, IN_PROJ_WIDTH), D_MODEL ** -0.5),
        "conv_w": nrm(ks[4], (DEPTH, CONV_K, CONV_WIDTH), CONV_K ** -0.5),
        "lambda_q1": nrm(ks[5], (DEPTH, ATTN_HEAD_DIM), 0.1),
        "lambda_k1": nrm(ks[6], (DEPTH, ATTN_HEAD_DIM), 0.1),
        "lambda_q2": nrm(ks[7], (DEPTH, ATTN_HEAD_DIM), 0.1),
        "lambda_k2": nrm(ks[8], (DEPTH, ATTN_HEAD_DIM), 0.1),
        "attn_subln_g": gain(ks[9], (DEPTH, ATTN_VALUE_DIM)),
        "w_out": nrm(ks[10], (DEPTH, MIX_WIDTH, D_MODEL), MIX_WIDTH ** -0.5),
        "ffn_norm_g": gain(ks[11], (DEPTH, D_MODEL)),
        "w_router": nrm(ks[12], (DEPTH, D_MODEL, N_EXPERTS), D_MODEL ** -0.5),
        "w_gate": nrm(ks[13], (DEPTH, N_EXPERTS, D_MODEL, D_FF_EXPERT), D_MODEL ** -0.5),
        "w_up": nrm(ks[14], (DEPTH, N_EXPERTS, D_MODEL, D_FF_EXPERT), D_MODEL ** -0.5),
        "w_down": nrm(ks[15], (DEPTH, N_EXPERTS, D_FF_EXPERT, D_MODEL), D_FF_EXPERT ** -0.5),
        "final_norm_g": gain(ks[16], (D_MODEL,)),
    }


def reference(x, meta_tokens, mix_norm_g, w_in, conv_w, lambda_q1, lambda_k1, lambda_q2,
              lambda_k2, attn_subln_g, w_out, ffn_norm_g, w_router, w_gate, w_up, w_down,
              final_norm_g):
    B = x.shape[0]
    meta = jnp.broadcast_to(meta_tokens.astype(x.dtype)[None], (B, N_META, D_MODEL))
    h = jnp.concatenate([meta, x], axis=1)
    L = h.shape[1]
    Lp = -(-L // Q_BLOCK) * Q_BLOCK
    cos, sin = rope_tables(Lp)
    splits = [ATTN_WIDTH, 2 * ATTN_WIDTH, 3 * ATTN_WIDTH,
              3 * ATTN_WIDTH + CONV_WIDTH, 3 * ATTN_WIDTH + 2 * CONV_WIDTH]

    for l in range(DEPTH):
        lam_init = 0.8 - 0.6 * math.exp(-0.3 * l)
        hn = rms_norm(h, mix_norm_g[l])
        proj = jnp.einsum('bld,dp->blp', hn, w_in[l])
        q, k, v, cx, cb, cc = jnp.split(proj, splits, axis=-1)
        attn = diff_attention_group(q, k, v, lambda_q1[l], lambda_k1[l], lambda_q2[l],
                                    lambda_k2[l], attn_subln_g[l], lam_init, cos, sin)
        conv = cb * centred_depthwise_conv(cc * cx, conv_w[l])
        mixed = jnp.concatenate([attn, conv.astype(attn.dtype)], axis=-1)
        h = h + jnp.einsum('blm,md->bld', mixed, w_out[l])
        h = h + expert_choice_ffn(rms_norm(h, ffn_norm_g[l]), w_router[l], w_gate[l],
                                  w_up[l], w_down[l])

    y = rms_norm(h, final_norm_g)
    return y[:, N_META:, :]
```

```python
import contextlib
import math
import numpy as np
import concourse.bass as bass
import concourse.mybir as mybir
from concourse.bass_utils import run_bass_kernel_spmd

F32 = mybir.dt.float32
BF16 = mybir.dt.bfloat16
I32 = mybir.dt.int32
ALU = mybir.AluOpType
AF = mybir.ActivationFunctionType

D = 1024
SEQ = 8192
NMETA = 16
L = SEQ + NMETA
LP = 8320
NT = 65
NE = 16
CAP = 1026
CAPP = 1152
NCT = 9
DFF = 2048
ROWS = L + 128
EPS = 1e-6
LAM_INIT = 0.8 - 0.6 * math.exp(-0.3 * 0)
BIG = 5000.0
NBIS = 28
ARENA_WORDS = 53100


def _size(dt):
    return 2 if dt == BF16 else 4


class DSem:
    def __init__(self, h):
        self.h = h
        self.total = 0


class Buf:
    __slots__ = ("w", "r")

    def __init__(self):
        self.w = []
        self.r = []


class Prog:
    ENG = ("pe", "act", "dve", "pool", "sp")

    def __init__(self, nc, es):
        self.nc = nc
        self.es = es
        self.q = {k: [] for k in self.ENG}
        self.cnt = {k: 0 for k in ("pe", "act", "dve", "pool")}
        self.sem = {k: es.enter_context(nc.semaphore("s_" + k)) for k in self.cnt}
        self.dsems = []

    def new_dsem(self):
        h = self.es.enter_context(self.nc.semaphore("d%d" % len(self.dsems)))
        d = DSem(h)
        self.dsems.append(d)
        return d

    @staticmethod
    def _deps(reads, writes, extra):
        deps = list(extra)
        for b in reads:
            deps += b.w
        for b in writes:
            deps += b.w
            deps += b.r
        return deps

    @staticmethod
    def _commit(tok, reads, writes):
        for b in writes:
            b.w = [tok]
            b.r = []
        for b in reads:
            b.r = [t for t in b.r if t[0] != tok[0]] + [tok]

    def op(self, eng, fn, reads=(), writes=(), extra=()):
        deps = self._deps(reads, writes, extra)
        self.cnt[eng] += 1
        tok = (eng, self.cnt[eng])
        self.q[eng].append((fn, deps, tok, 1))
        self._commit(tok, reads, writes)
        return tok

    def dma(self, queue, fn, dsem, reads=(), writes=(), extra=()):
        deps = self._deps(reads, writes, extra)
        dsem.total += 16
        tok = (dsem, dsem.total)
        self.q[queue].append((fn, deps, tok, 16))
        self._commit(tok, reads, writes)
        return tok

    def barrier(self):
        toks = [(k, self.cnt[k]) for k in self.cnt if self.cnt[k] > 0]
        toks += [(d, d.total) for d in self.dsems if d.total > 0]
        for k in self.ENG:
            self.q[k].append((None, toks, None, 0))

    def barrier_on(self, toks):
        for k in self.ENG:
            self.q[k].append((None, list(toks), None, 0))

    def load1(self, out, in_, b):
        return self.dma("sp", I_dma(out, in_), self.new_dsem(), writes=[b])

    def _h(self, k):
        return self.sem[k] if isinstance(k, str) else k.h

    def replay(self, name, e):
        waited = {}
        for fn, deps, tok, inc in self.q[name]:
            need = {}
            for k, v in deps:
                if name == "pe" and k == "pe" and fn is not None:
                    continue
                if v > need.get(k, 0):
                    need[k] = v
            for k, v in need.items():
                if waited.get(k, 0) < v:
                    e.wait_ge(self._h(k), v)
                    waited[k] = v
            if fn is None:
                continue
            ins = fn(e)
            ins.then_inc(self._h(tok[0]), inc)


class Arena:
    def __init__(self, t, nwords):
        self.t = t
        self.n = nwords
        self.top = 0

    def alloc_top(self, shape, dt=F32):
        nel = int(np.prod(shape))
        nw = ((nel * _size(dt) + 3) // 4 + 7) // 8 * 8
        self.n -= nw
        save = self.top
        self.top = self.n
        self.n += nw
        a = self.alloc(shape, dt)
        self.n -= nw
        self.top = save
        assert self.top <= self.n
        return a

    def alloc(self, shape, dt=F32):
        nel = int(np.prod(shape))
        nw = (nel * _size(dt) + 3) // 4
        nw = (nw + 7) // 8 * 8
        assert self.top + nw <= self.n, ("arena overflow", self.top, nw)
        a = self.t[:, self.top:self.top + nw]
        self.top += nw
        if dt != F32:
            a = a.bitcast(dt)
        a = a[:, 0:nel]
        if len(shape) == 2:
            a = a.rearrange("p (a b) -> p a b", b=shape[1])
        elif len(shape) == 3:
            a = a.rearrange("p (a b c) -> p a b c", b=shape[1], c=shape[2])
        return a


def I_dma(out, in_):
    return lambda e: e.dma_start(out=out, in_=in_)


def I_dma_nc(out, in_):
    return lambda e: e.dma_start(out=out, in_=in_, allow_slow_non_contiguous=True)


_REGS = {}


def bc_reg(e, v):
    if v not in _REGS:
        _REGS[v] = e.to_reg(v)
    return _REGS[v]


def I_act(out, in_, func, **kw):
    return lambda e: e.activation(out=out, in_=in_, func=func, **kw)


def I_tt(out, a, b, op):
    return lambda e: e.tensor_tensor(out=out, in0=a, in1=b, op=op)


def I_ts(out, a, s1, s2, op0, op1=None, accum=None):
    def f(e):
        kw = {}
        if op1 is not None:
            kw["op1"] = op1
        if accum is not None:
            kw["accum_out"] = accum
        return e.tensor_scalar(out=out, in0=a, scalar1=s1, scalar2=s2, op0=op0, **kw)
    return f


def I_stt(out, a, sc, b, op0, op1, accum=None):
    def f(e):
        kw = {}
        if accum is not None:
            kw["accum_out"] = accum
        return e.scalar_tensor_tensor(out=out, in0=a, scalar=sc, in1=b, op0=op0, op1=op1, **kw)
    return f


def I_copy(out, in_):
    return lambda e: e.tensor_copy(out=out, in_=in_)


def I_acopy(out, in_):
    return lambda e: e.copy(out=out, in_=in_)


def I_memset(ap, v):
    return lambda e: e.memset(ap, v)


def I_recip(out, in_):
    return lambda e: e.reciprocal(out=out, in_=in_)


def I_mm(lst):
    def f(e):
        ins = None
        for (out, lhsT, rhs, st, sp, kw) in lst:
            ins = e.matmul(out, lhsT=lhsT, rhs=rhs, start=st, stop=sp, **kw)
        return ins
    return f


def I_trs(lst):
    def f(e):
        ins = None
        for (out, in_, ident) in lst:
            ins = e.transpose(out=out, in_=in_, identity=ident)
        return ins
    return f


DEBUG = []


def build_nc():
    _REGS.clear()
    nc = bass.Bass("TRN2", target_bir_lowering=False)

    def din(name, shape, dt=F32):
        return nc.dram_tensor(name, shape, dt, kind="ExternalInput").ap()

    def dint(name, shape, dt):
        kind = "ExternalOutput" if name in DEBUG else "Internal"
        return nc.dram_tensor(name, shape, dt, kind=kind).ap()

    x_d = din("x", [SEQ, D])
    meta_d = din("meta", [NMETA, D])
    g1_d = din("g1b", [128, D])
    g2_d = din("g2b", [128, D])
    g3_d = din("g3b", [128, D])
    win_d = din("w_in", [D, 3072])
    wout_d = din("w_out", [D, D])
    wr_d = din("w_router", [D, NE])
    wg_d = din("w_gate", [NE, D, DFF])
    wu_d = din("w_up", [NE, D, DFF])
    wd_d = din("w_down", [NE, DFF, D])
    convw_d = din("convw", [128, 4, 3])
    lamp_d = din("lamp", [128, 4, 64])
    gsub_d = din("gsub", [128, 128])
    ropeq_d = din("ropeq", [LP, 128])
    ropek_d = din("ropek", [LP, 128])
    ident_d = din("ident", [128, 128])
    tokid_d = din("tokid", [128, NT])
    dummy_d = din("dummyrows", [128, NCT, 2])
    gmat_d = din("gmat", [128, 128])
    tmat_d = din("tmat", [128, 128])
    out_d = nc.dram_tensor("out", [SEQ, D], F32, kind="ExternalOutput").ap()

    qT_d = dint("qT_s", [4, 128, LP], BF16)
    kT_d = dint("kT_s", [4, 128, LP], BF16)
    v_d = dint("v_s", [4, 128, NT, 128], BF16)
    uT_d = dint("uT_s", [4, 128, L + 2], F32)
    cbT_d = dint("cbT_s", [4, 128, LP], F32)
    attnT_d = dint("attnT_s", [4, 128, LP], BF16)
    convT_d = dint("convT_s", [4, 128, LP], BF16)
    aff_d = dint("aff_s", [NE, L], F32)
    hacc_d = dint("hacc_s", [ROWS, D], F32)
    hs_d = dint("hs_s", [ROWS, D], BF16)
    comb_d = [dint("comb_s%d" % e, [CAPP, 2], F32) for e in range(NE)]
    h1dbg_d = dint("h1dbg_s", [LP, D], F32) if "h1dbg_s" in DEBUG else None

    es = contextlib.ExitStack()
    with es:
        arena_t = es.enter_context(nc.sbuf_tensor("arena", [128, ARENA_WORDS], F32))
        ps = es.enter_context(nc.psum_tensor("ps", [128, 4096], F32))
        p = Prog(nc, es)
        ar = Arena(arena_t, ARENA_WORDS)

        def bank(b, n=1):
            return ps[:, b * 512:(b + n) * 512]

        ident_f = ar.alloc([128])
        ident_b = ar.alloc([128], BF16)
        cols = ar.alloc([64])
        lam_col = cols[:, 0:1]
        zt = ar.alloc([1024])
        misc_ld = p.new_dsem()
        misc_st = p.new_dsem()
        b_ident = Buf()
        b_lam = Buf()
        b_z = Buf()

        p.load1(ident_f, ident_d, b_ident)
        p.op("dve", I_copy(ident_b, ident_f), reads=[b_ident], writes=[b_ident])
        p.op("pool", I_memset(zt, 0.0), writes=[b_z])

        lamp = ar.alloc([4, 64])
        ljunk = ar.alloc([64])
        b_lp = Buf()
        p.load1(lamp, lamp_d, b_lp)
        p.op("dve", I_stt(ljunk, lamp[:, 0, :], 1.0, lamp[:, 1, :], ALU.mult, ALU.mult, accum=cols[:, 1:2]),
             reads=[b_lp], writes=[b_lam])
        p.op("dve", I_stt(ljunk, lamp[:, 2, :], 1.0, lamp[:, 3, :], ALU.mult, ALU.mult, accum=cols[:, 2:3]),
             reads=[b_lp, b_lam], writes=[b_lam])
        p.op("act", I_act(cols[:, 3:5], cols[:, 1:3], AF.Exp), reads=[b_lam], writes=[b_lam])
        p.op("dve", I_tt(cols[:, 5:6], cols[:, 3:4], cols[:, 4:5], ALU.subtract), reads=[b_lam], writes=[b_lam])
        p.op("dve", I_ts(lam_col, cols[:, 5:6], float(LAM_INIT), None, ALU.add), reads=[b_lam], writes=[b_lam])

        dm = ar.alloc([NCT, 2])
        comb = ar.alloc([NT, NE, 2])
        slot_i = ar.alloc([NT, NE], I32)
        b_dm = Buf()

        def emit_scratch_init():
            p.dma("sp", I_dma_nc(uT_d[:, :, 0:1].rearrange("j p o -> p j o"), zt[:, 0:4].unsqueeze(2)), misc_st,
                  reads=[b_z])
            p.dma("sp", I_dma_nc(uT_d[:, :, L + 1:L + 2].rearrange("j p o -> p j o"), zt[:, 0:4].unsqueeze(2)),
                  misc_st, reads=[b_z])
            p.dma("sp", I_dma(hs_d[LP:ROWS, :], zt[0:16, 0:512].bitcast(BF16)), misc_st, reads=[b_z])
            p.dma("sp", I_dma(hacc_d[LP:ROWS, :], zt[0:16, :]), misc_st, reads=[b_z])
            p.load1(dm, dummy_d, b_dm)
            for e_ in range(NE):
                p.dma("sp", I_dma(comb_d[e_].rearrange("(ct p) t -> p ct t", p=128), dm), misc_st, reads=[b_dm])

        persist_top = ar.top

        def rstd_chain(ss_ap, n, tmp_ap, out_ap, b_ss, b_tmp, b_out):
            p.op("dve", I_ts(tmp_ap, ss_ap, 1.0 / n, EPS, ALU.mult, ALU.add), reads=[b_ss], writes=[b_tmp])
            p.op("act", I_act(tmp_ap, tmp_ap, AF.Ln), reads=[], writes=[b_tmp])
            p.op("act", I_act(out_ap, tmp_ap, AF.Exp, scale=-0.5), reads=[b_tmp], writes=[b_out])

        win_sb = ar.alloc([8, 3072], BF16)
        g1b = ar.alloc([1024])
        b_win = Buf()
        b_g = Buf()
        wsem = p.new_dsem()
        winv = win_d.rearrange("(c p) n -> p c n", p=128)
        b_winf = Buf()
        wsemf = p.new_dsem()
        for c in range(8):
            p.dma("pool", I_dma(win_sb[:, c, 1536:3072], winv[:, c, 1536:3072]), wsemf)
        b_winf.w = [(wsemf, wsemf.total)]
        for c in range(8):
            p.dma("pool", I_dma(win_sb[:, c, 0:1536], winv[:, c, 0:1536]), wsem)
        b_win.w = [(wsem, wsem.total)]
        p.load1(g1b, g1_d, b_g)

        xt = [ar.alloc([4, 1024]) for _ in range(2)]
        rtq = ar.alloc([4, 128])
        rtk = ar.alloc([4, 128])
        c1 = ar.alloc([16])
        junkb = ar.alloc([1024], BF16)
        hn = ar.alloc([4, 1024], BF16)
        hnT = [ar.alloc([8, 512], BF16) for _ in range(2)]
        cx_sb = ar.alloc([512])
        uT_st = ar.alloc([4, 512])
        cbT_st = ar.alloc([4, 512])
        rt1 = ar.alloc([8, 64])
        rt2 = ar.alloc([8, 64])
        qk_tm = ar.alloc([4, 2, 512], BF16)
        v_st = ar.alloc([4, 512], BF16)
        qkT_st = ar.alloc([8, 512], BF16)

        b_xt = [Buf(), Buf()]
        b_rt = Buf()
        b_ss = Buf(); b_tmp = Buf(); b_rstd = Buf()
        b_junk = Buf()
        b_hn = [Buf() for _ in range(4)]
        b_hnT = [[Buf() for _ in range(4)] for _ in range(2)]
        b_cx = Buf(); b_uT = Buf(); b_cbT = Buf()
        b_r1 = Buf(); b_r2 = Buf()
        b_qktm = [[Buf() for _ in range(2)] for _ in range(4)]
        b_vst = Buf(); b_qkT = Buf()
        b_ps = [Buf() for _ in range(8)]
        xsem = [p.new_dsem(), p.new_dsem()]
        rsem = p.new_dsem()
        st_u = p.new_dsem(); st_cb = p.new_dsem(); st_qk = p.new_dsem(); st_v = p.new_dsem()

        def load_x(T, bufs, xtile, sems, bxt):
            nsub = 4 if T < 16 else 1
            n0 = T * 512
            if T < 16:
                p.dma("sp", I_dma(xtile[T % 2], x_d[n0:n0 + 512, :].rearrange("(s p) d -> p s d", p=128)),
                      sems[T % 2], writes=[bxt[T % 2]])
            else:
                tokm = p.op("pool", I_memset(xtile[T % 2][:, 0, :], 0.0), writes=[bxt[T % 2]])
                tokd = p.dma("sp", I_dma(xtile[T % 2][0:16, 0, :], meta_d), sems[T % 2], extra=[tokm])
                bxt[T % 2].w.append(tokd)

        NSUP = 17
        rtq2 = [rtq, ar.alloc([4, 128])]
        rtk2 = [rtk, ar.alloc([4, 128])]
        b_rt2 = [Buf(), Buf()]
        rsem2 = [p.new_dsem(), p.new_dsem()]

        def stageA(T):
            nsub = 4 if T < 16 else 1
            N = nsub * 128
            n0 = T * 512
            X = xt[T % 2]
            bX = b_xt[T % 2]
            HT = hnT[T % 2]
            bHT = b_hnT[T % 2]
            p.dma("sp", I_dma(rtq2[T % 2][:, 0:nsub, :], ropeq_d[n0:n0 + N, :].rearrange("(s p) f -> p s f", p=128)),
                  rsem2[T % 2], writes=[b_rt2[T % 2]])
            tk_ = p.dma("sp", I_dma(rtk2[T % 2][:, 0:nsub, :],
                                    ropek_d[n0:n0 + N, :].rearrange("(s p) f -> p s f", p=128)), rsem2[T % 2])
            b_rt2[T % 2].w.append(tk_)
            for s in range(nsub):
                p.op("act", I_act(junkb, X[:, s, :], AF.Square, accum_out=c1[:, s:s + 1]),
                     reads=[bX], writes=[b_junk, b_ss] if s == 0 else [b_junk])
            b_ss.w = [("act", p.cnt["act"])]
            rstd_chain(c1[:, 0:nsub], float(D), c1[:, 4:4 + nsub], c1[:, 8:8 + nsub], b_ss, b_tmp, b_rstd)
            for s in range(nsub):
                p.op("dve", I_stt(hn[:, s, :], X[:, s, :], c1[:, 8 + s:9 + s], g1b, ALU.mult, ALU.mult),
                     reads=[bX, b_rstd, b_g], writes=[b_hn[s]])
            for s in range(nsub):
                pb = bank(s % 2).bitcast(BF16).rearrange("p (c t) -> p c t", t=128)
                p.op("pe", I_trs([(pb[:, c, :], hn[:, s, c * 128:(c + 1) * 128], ident_b) for c in range(8)]),
                     reads=[b_hn[s], b_ident], writes=[b_ps[s % 2]])
                p.op("act" if s % 2 == 0 else "dve",
                     (I_acopy if s % 2 == 0 else I_copy)(HT[:, :, s * 128:(s + 1) * 128], pb),
                     reads=[b_ps[s % 2]], writes=[bHT[s]])

        def chunksB(T):
            nsub = 4 if T < 16 else 1
            N = nsub * 128
            n0 = T * 512
            HT = hnT[T % 2]
            bHT = b_hnT[T % 2]
            rHT = [bHT[s] for s in range(nsub)]
            rtqT = rtq2[T % 2]
            rtkT = rtk2[T % 2]
            bRT = b_rt2[T % 2]
            out = []
            fmc = [0]

            def fm_chunk(j):
                def fmm(col0, pbk):
                    return I_mm([(pbk[:, 0:N], win_sb[:, c, col0:col0 + 128], HT[:, c, 0:N], c == 0, c == 7, {})
                                 for c in range(8)])
                pcx = bank(2 + fmc[0] % 2); bcx = b_ps[2 + fmc[0] % 2]; fmc[0] += 1
                p.op("pe", fmm(1536 + j * 128, pcx), reads=rHT + [b_winf], writes=[bcx])
                p.op("act", I_acopy(cx_sb[:, 0:N], pcx[:, 0:N]), reads=[bcx], writes=[b_cx])
                pcc = bank(2 + fmc[0] % 2); bcc = b_ps[2 + fmc[0] % 2]; fmc[0] += 1
                p.op("pe", fmm(2560 + j * 128, pcc), reads=rHT + [b_winf], writes=[bcc])
                p.op("dve", I_tt(uT_st[:, j, 0:N], pcc[:, 0:N], cx_sb[:, 0:N], ALU.mult),
                     reads=[bcc, b_cx], writes=[b_uT] if j == 0 else [])
                pcb = bank(2 + fmc[0] % 2); bcb = b_ps[2 + fmc[0] % 2]; fmc[0] += 1
                p.op("pe", fmm(2048 + j * 128, pcb), reads=rHT + [b_winf], writes=[bcb])
                p.op("act", I_acopy(cbT_st[:, j, 0:N], pcb[:, 0:N]), reads=[bcb], writes=[b_cbT] if j == 0 else [])
                if j == 3:
                    b_uT.w = [("dve", p.cnt["dve"])]
                    b_cbT.w = [("act", p.cnt["act"])]
                    NV = N if T < 16 else NMETA
                    ucol = (17 + n0) if T < 16 else 1
                    p.dma("pool", I_dma(uT_d[:, :, ucol:ucol + NV].rearrange("j p n -> p j n"), uT_st[:, :, 0:NV]),
                          st_u, reads=[b_uT])
                    p.dma("pool", I_dma(cbT_d[:, :, n0:n0 + N].rearrange("j p n -> p j n"), cbT_st[:, :, 0:N]),
                          st_cb, reads=[b_cbT])

            for j in range(4):
                out.append(lambda j=j: fm_chunk(j))

            tmc = [0]
            deferred = []
            state = {"first_qk": True, "first_v": True}

            def qk_transposes(s, grp):
                pq = bank(6 + (s * 2 + grp) % 2).bitcast(BF16)[:, 0:512].rearrange("p (h t) -> p h t", t=128)
                bpq = b_ps[6 + (s * 2 + grp) % 2]
                p.op("pe", I_trs([(pq[:, h, :], qk_tm[:, s, grp, h * 128:(h + 1) * 128], ident_b)
                                  for h in range(4)]),
                     reads=[b_qktm[s][grp], b_ident], writes=[bpq])
                p.op("act", I_acopy(qkT_st[:, grp * 4:(grp + 1) * 4, s * 128:(s + 1) * 128], pq),
                     reads=[bpq], writes=[b_qkT] if state["first_qk"] else [])
                state["first_qk"] = False

            def tm_chunk(s, grp):
                pt = bank(4 + tmc[0] % 2); bpt = b_ps[4 + tmc[0] % 2]; tmc[0] += 1
                p.op("pe", I_mm([(pt, HT[:, c, s * 128:(s + 1) * 128], win_sb[:, c, grp * 512:(grp + 1) * 512],
                                  c == 0, c == 7, {}) for c in range(8)]),
                     reads=[bHT[s], b_win], writes=[bpt])
                if grp == 2:
                    p.op("act", I_acopy(v_st[:, s, :], pt), reads=[bpt], writes=[b_vst] if state["first_v"] else [])
                    state["first_v"] = False
                else:
                    rt = rtqT if grp == 0 else rtkT
                    psq = pt.rearrange("p (g d) -> p g d", d=64)
                    c2b = rt[:, s, 0:64].unsqueeze(1).to_broadcast([128, 8, 64])
                    nsb = rt[:, s, 64:96].unsqueeze(1).to_broadcast([128, 8, 32])
                    psb = rt[:, s, 96:128].unsqueeze(1).to_broadcast([128, 8, 32])
                    p.op("dve", I_tt(rt1, psq, c2b, ALU.mult), reads=[bpt, bRT], writes=[b_r1])
                    p.op("dve", I_tt(rt2[:, :, 0:32], psq[:, :, 32:64], nsb, ALU.mult), reads=[bpt, bRT],
                         writes=[b_r2])
                    tk2 = p.op("dve", I_tt(rt2[:, :, 32:64], psq[:, :, 0:32], psb, ALU.mult), reads=[bpt, bRT])
                    b_r2.w.append(tk2)
                    dst = qk_tm[:, s, grp, :].rearrange("p (g d) -> p g d", d=64)
                    p.op("dve", I_tt(dst, rt1, rt2, ALU.add), reads=[b_r1, b_r2], writes=[b_qktm[s][grp]])
                    deferred.append((s, grp))
                while len(deferred) > 2:
                    qk_transposes(*deferred.pop(0))

            for s in range(nsub):
                for grp in range(3):
                    out.append(lambda s=s, grp=grp: tm_chunk(s, grp))

            def finish():
                while deferred:
                    qk_transposes(*deferred.pop(0))
                b_qkT.w = [("act", p.cnt["act"])]
                b_vst.w = [("act", p.cnt["act"])]
                p.dma("pool", I_dma(qT_d[:, :, n0:n0 + N].rearrange("h p n -> p h n"), qkT_st[:, 0:4, 0:N]), st_qk,
                      reads=[b_qkT])
                p.dma("pool", I_dma(kT_d[:, :, n0:n0 + N].rearrange("h p n -> p h n"), qkT_st[:, 4:8, 0:N]), st_qk,
                      reads=[b_qkT])
                for h in range(4):
                    p.dma("pool", I_dma(v_d[h, :, T * 4:T * 4 + nsub, :], v_st[:, 0:nsub, h * 128:(h + 1) * 128]),
                          st_v, reads=[b_vst])
            out.append(finish)
            return out

        load_x(0, None, xt, xsem, b_xt)
        stageA(0)
        for T in range(NSUP):
            if T + 1 < NSUP:
                load_x(T + 1, None, xt, xsem, b_xt)
            ch = chunksB(T)
            half = 5
            for f_ in ch[:half]:
                f_()
            if T + 1 < NSUP:
                stageA(T + 1)
            for f_ in ch[half:]:
                f_()
        p.barrier()
        ar.top = persist_top

        QT = [ar.alloc([LP], BF16) for _ in range(2)]
        KT = [ar.alloc([LP], BF16) for _ in range(2)]
        VA = [ar.alloc([NT, 129], BF16) for _ in range(2)]
        Eb = [ar.alloc([2, 512], BF16) for _ in range(3)]
        gsub_b = ar.alloc([128])
        tmp_t = ar.alloc([128])
        o_s = ar.alloc([4, 128])
        on_s = ar.alloc([4, 128], BF16)
        jk = ar.alloc([128])
        c2 = ar.alloc([32])
        accs = ar.alloc([3, 408])
        aT_st = [ar.alloc([512], BF16) for _ in range(2)]
        b_qkv = [Buf(), Buf()]
        b_E = [Buf() for _ in range(3)]
        b_S = [Buf(), Buf()]
        b_acc = Buf()
        b_gs = Buf()
        b_c2 = Buf(); b_tt = Buf(); b_o = Buf(); b_on = Buf(); b_jk = Buf(); b_ms = Buf(); b_ms2 = Buf(); b_rs = Buf()
        b_accs = Buf()
        b_p7 = Buf()
        b_aT = [Buf(), Buf()]
        hsem = [p.new_dsem(), p.new_dsem()]
        st2 = [p.new_dsem(), p.new_dsem()]

        p.load1(gsub_b, gsub_d, b_gs)
        p.op("dve", I_ts(gsub_b, gsub_b, float(1.0 - LAM_INIT), None, ALU.mult), reads=[], writes=[b_gs])

        convw = ar.alloc([4, 3])
        cut = [ar.alloc([4, 514]) for _ in range(2)]
        ccb = [ar.alloc([4, 512]) for _ in range(2)]
        cca = ar.alloc([512])
        ccb2 = ar.alloc([512])
        cv_st = [ar.alloc([4, 512], BF16) for _ in range(2)]
        b_cw = Buf(); b_cut = [Buf(), Buf()]; b_ccb = [Buf(), Buf()]; b_cca = Buf(); b_ccb2 = Buf()
        b_cv = [Buf(), Buf()]
        ld_cut = [p.new_dsem(), p.new_dsem()]
        ld_ccb = [p.new_dsem(), p.new_dsem()]
        st_cv = [p.new_dsem(), p.new_dsem()]
        p.load1(convw, convw_d, b_cw)
        p.op("dve", I_memset(cv_st[0], 0.0), writes=[b_cv[0]])
        p.op("dve", I_memset(cv_st[1], 0.0), writes=[b_cv[1]])

        def conv_load(T):
            nsub = 4 if T < 16 else 1
            N = nsub * 128
            n0 = T * 512
            NV = N if T < 16 else NMETA
            sl = T % 2
            ucol = (16 + n0) if T < 16 else 0
            p.dma("sp", I_dma(cut[sl][:, :, 0:NV + 2], uT_d[:, :, ucol:ucol + NV + 2].rearrange("j p n -> p j n")),
                  ld_cut[sl], writes=[b_cut[sl]])
            p.dma("sp", I_dma(ccb[sl][:, :, 0:N], cbT_d[:, :, n0:n0 + N].rearrange("j p n -> p j n")), ld_ccb[sl],
                  writes=[b_ccb[sl]])

        def conv_compute(T):
            nsub = 4 if T < 16 else 1
            N = nsub * 128
            n0 = T * 512
            NV = N if T < 16 else NMETA
            sl = T % 2
            U = cut[sl]
            for j in range(4):
                p.op("dve", I_ts(cca[:, 0:NV], U[:, j, 0:NV], convw[:, j, 0:1], None, ALU.mult),
                     reads=[b_cut[sl], b_cw], writes=[b_cca])
                p.op("dve", I_stt(ccb2[:, 0:NV], U[:, j, 1:NV + 1], convw[:, j, 1:2], cca[:, 0:NV], ALU.mult, ALU.add),
                     reads=[b_cut[sl], b_cca], writes=[b_ccb2])
                p.op("dve", I_stt(cca[:, 0:NV], U[:, j, 2:NV + 2], convw[:, j, 2:3], ccb2[:, 0:NV], ALU.mult, ALU.add),
                     reads=[b_cut[sl], b_ccb2], writes=[b_cca])
                p.op("dve", I_tt(cv_st[sl][:, j, 0:NV], cca[:, 0:NV], ccb[sl][:, j, 0:NV], ALU.mult),
                     reads=[b_cca, b_ccb[sl]], writes=[b_cv[sl]] if j == 0 else [])
            b_cv[sl].w = [("dve", p.cnt["dve"])]
            p.dma("pool", I_dma(convT_d[:, :, n0:n0 + N].rearrange("j p n -> p j n"), cv_st[sl][:, :, 0:N]), st_cv[sl],
                  reads=[b_cv[sl]])
        for sl in range(2):
            p.op("pool", I_memset(VA[sl][:, :, 128:129], 1.0), writes=[b_qkv[sl]])
            p.op("pool", I_memset(VA[sl][:, NT - 1, 128:129], 0.0), writes=[b_qkv[sl]])
            p.op("pool", I_memset(VA[sl][0:16, NT - 1, 128:129], 1.0), writes=[b_qkv[sl]])

        def load_head(h):
            sl = h % 2
            p.dma("sp", I_dma(QT[sl], qT_d[h]), hsem[sl], writes=[b_qkv[sl]])
            t1_ = p.dma("sp", I_dma(KT[sl], kT_d[h]), hsem[sl])
            t2_ = p.dma("sp", I_dma(VA[sl][:, :, 0:128], v_d[h]), hsem[sl])
            b_qkv[sl].w += [t1_, t2_]

        def S2(sb):
            return bank(2 * sb, 2).rearrange("p (a c) -> p a c", c=512)

        def acc_ap(i, s, nsub=4):
            a = i * nsub + s
            bk = bank(4 + a // 3)
            o = (a % 3) * 129
            return bk[:, o:o + 129]

        items = [(h, qb, kt) for h in range(4) for qb in range(17) for kt in range(NT)]

        Qp = [ar.alloc([2, 512], BF16) for _ in range(2)]
        b_qp = [Buf(), Buf()]
        for sl_ in range(2):
            p.op("dve", I_memset(Qp[sl_], 0.0), writes=[b_qp[sl_]])

        def emit_qpad(h, qb):
            sl = h % 2
            qs = (h * 17 + qb) % 2
            q0 = qb * 512
            nq = 512 if qb < 16 else 128
            p.op("dve", I_copy(Qp[qs][0:64, 0, 0:nq], QT[sl][0:64, q0:q0 + nq]), reads=[b_qkv[sl]],
                 writes=[b_qp[qs]])
            tk_ = p.op("dve", I_copy(Qp[qs][64:128, 1, 0:nq], QT[sl][64:128, q0:q0 + nq]), reads=[b_qkv[sl]])
            b_qp[qs].w.append(tk_)

        def emit_qk(idx):
            h, qb, kt = items[idx]
            sl = h % 2
            sb = idx % 2
            qs = (h * 17 + qb) % 2
            nq = 512 if qb < 16 else 128
            s2 = S2(sb)
            p.op("pe", I_mm([
                (s2[:, 0, 0:nq], KT[sl][:, kt * 128:(kt + 1) * 128], Qp[qs][:, 0, 0:nq], True, True, {}),
                (s2[:, 1, 0:nq], KT[sl][:, kt * 128:(kt + 1) * 128], Qp[qs][:, 1, 0:nq], True, True, {})]),
                 reads=[b_qkv[sl], b_qp[qs]], writes=[b_S[sb]])

        def emit_exp_av(idx, nxt=None):
            h, qb, kt = items[idx]
            sl = h % 2
            sb = idx % 2
            eb = idx % 3
            nq = 512 if qb < 16 else 128
            nsub = nq // 128
            na = nq if qb < 16 else NMETA
            p.op("act", I_act(Eb[eb][:, :, 0:na], S2(sb)[:, :, 0:na], AF.Exp), reads=[b_S[sb]], writes=[b_E[eb]])
            if nxt is not None:
                emit_qk(nxt)
            lst = []
            seen = set()
            for i in range(2):
                for s in range(nsub):
                    bk_ = (i * nsub + s) // 3
                    st_ = (kt == 0) and (bk_ not in seen)
                    seen.add(bk_)
                    lst.append((acc_ap(i, s, nsub), Eb[eb][:, i, s * 128:(s + 1) * 128], VA[sl][:, kt, :],
                                st_, kt == NT - 1, dict(skip_group_check=True)))
            extra = list(b_acc.r) if kt == 0 else []
            tok = p.op("pe", I_mm(lst), reads=[b_E[eb], b_qkv[sl]], extra=extra)
            if kt == NT - 1:
                b_acc.w = [tok]
                b_acc.r = []

        epi_cnt = [0]
        pending = []

        def acc_sb(i, s_, nsub=4):
            a_ = i * nsub + s_
            o_ = (a_ % 3) * 129
            return accs[:, a_ // 3, o_:o_ + 129]

        def emit_epilogue(h, qb, idx):
            q0 = qb * 512
            nq = 512 if qb < 16 else 128
            nsub = nq // 128
            sts = epi_cnt[0] % 2
            epi_cnt[0] += 1
            nacc = 2 * nsub
            toksA = []
            for bk in range((nacc + 2) // 3):
                w_ = 129 * min(3, nacc - 3 * bk)
                toksA.append(p.op("dve", I_copy(accs[:, bk, 0:w_], bank(4 + bk)[:, 0:w_]), reads=[b_acc],
                                  writes=[b_accs] if bk == 0 else []))
            b_accs.w = toksA
            for s_ in range(nsub):
                a0 = acc_sb(0, s_, nsub)
                a1 = acc_sb(1, s_, nsub)
                p.op("dve", I_recip(c2[:, 0:1], a0[:, 128:129]), reads=[b_accs], writes=[b_c2])
                p.op("dve", I_recip(c2[:, 1:2], a1[:, 128:129]), reads=[b_accs], writes=[b_c2])
                p.op("dve", I_tt(c2[:, 2:3], c2[:, 1:2], lam_col, ALU.mult), reads=[b_lam], writes=[b_c2])
                p.op("dve", I_ts(tmp_t, a1[:, 0:128], c2[:, 2:3], None, ALU.mult), reads=[b_accs, b_c2],
                     writes=[b_tt])
                p.op("dve", I_stt(o_s[:, s_, :], a0[:, 0:128], c2[:, 0:1], tmp_t, ALU.mult, ALU.subtract),
                     reads=[b_accs, b_c2, b_tt], writes=[b_o])
                p.op("dve", I_stt(jk, o_s[:, s_, :], 1.0, o_s[:, s_, :], ALU.mult, ALU.mult,
                                  accum=c2[:, 8 + s_:9 + s_]), reads=[b_o], writes=[b_jk, b_ms])
            p.op("dve", I_ts(c2[:, 12:12 + nsub], c2[:, 8:8 + nsub], 1.0 / 128.0, EPS, ALU.mult, ALU.add),
                 reads=[b_ms], writes=[b_ms2])

            def stage_cd():
                p.op("act", I_act(c2[:, 12:12 + nsub], c2[:, 12:12 + nsub], AF.Ln), writes=[b_ms2])
                p.op("act", I_act(c2[:, 16:16 + nsub], c2[:, 12:12 + nsub], AF.Exp, scale=-0.5), reads=[b_ms2],
                     writes=[b_rs])
                for s_ in range(nsub):
                    p.op("dve", I_stt(on_s[:, s_, :], o_s[:, s_, :], c2[:, 16 + s_:17 + s_], gsub_b, ALU.mult,
                                      ALU.mult), reads=[b_o, b_rs, b_gs], writes=[b_on])

            def stage_e():
                p7 = bank(7).bitcast(BF16)[:, 0:512].rearrange("p (s t) -> p s t", t=128)
                p.op("pe", I_trs([(p7[:, s_, :], on_s[:, s_, :], ident_b) for s_ in range(nsub)]),
                     reads=[b_on, b_ident], writes=[b_p7])
                p.op("dve", I_copy(aT_st[sts][:, 0:nq], bank(7).bitcast(BF16)[:, 0:nq]), reads=[b_p7],
                     writes=[b_aT[sts]])
                p.dma("pool", I_dma(attnT_d[h, :, q0:q0 + nq], aT_st[sts][:, 0:nq]), st2[sts], reads=[b_aT[sts]])

            pending.append((idx + 14, stage_cd))
            pending.append((idx + 26, stage_e))

        load_head(0)
        load_head(1)
        conv_load(0)
        emit_scratch_init()
        emit_qpad(0, 0)
        emit_qk(0)
        emit_qk(1)
        for idx in range(len(items)):
            h, qb, kt = items[idx]
            if kt == 0 and idx + NT < len(items):
                nh, nqb, _ = items[idx + NT]
                emit_qpad(nh, nqb)
            emit_exp_av(idx, idx + 2 if idx + 2 < len(items) else None)
            if h == 0 and kt == 5 and qb + 1 < NSUP:
                conv_load(qb + 1)
            if h == 0 and kt == 30:
                conv_compute(qb)
            while pending and pending[0][0] <= idx:
                pending.pop(0)[1]()
            if kt == NT - 1:
                emit_epilogue(h, qb, idx)
                if qb == 16 and h + 2 < 4:
                    load_head(h + 2)
        while pending:
            pending.pop(0)[1]()
        p.barrier()
        ar.top = persist_top

        wout_sb = ar.alloc([8, 1024], BF16)
        wr_sb = ar.alloc([8, 16])
        wr_hi = ar.alloc([8, 16], BF16)
        wr_lo = ar.alloc([8, 16], BF16)
        g2b = ar.alloc([1024])
        affT = ar.alloc([LP])
        tokid = ar.alloc([NT])
        p34_top = ar.top
        mixT = [ar.alloc([8, 512], BF16) for _ in range(2)]
        xt3 = [ar.alloc([4, 1024]) for _ in range(2)]
        h1 = [ar.alloc([1024]) for _ in range(4)]
        hsf = [ar.alloc([1024]) for _ in range(2)]
        hhi = [ar.alloc([1024], BF16) for _ in range(2)]
        hlo = [ar.alloc([1024], BF16) for _ in range(2)]
        hlT = [ar.alloc([16, 128], BF16) for _ in range(2)]
        c3 = ar.alloc([64])
        sqjunk = ar.alloc([1024], BF16)
        b_sqj = Buf()
        ex = [ar.alloc([16]) for _ in range(2)]

        b_wout = Buf(); b_wr = Buf(); b_g2 = Buf(); b_cw = Buf(); b_tok = Buf(); b_comb = Buf(); b_affT = Buf()
        b_mix = [Buf(), Buf()]
        b_mixc = [[Buf() for _ in range(4)] for _ in range(2)]
        b_ut = [Buf(), Buf()]; b_cbt = [Buf(), Buf()]; b_ca = Buf(); b_cb2 = Buf()
        b_xt3 = [Buf(), Buf()]
        b_h1 = [Buf() for _ in range(4)]
        b_hsf = [Buf(), Buf()]; b_hhi = [Buf(), Buf()]; b_hlo = [Buf(), Buf()]; b_hlT = [Buf(), Buf()]
        b_ss3 = [Buf(), Buf()]; b_t3 = [Buf(), Buf()]; b_r3 = [Buf(), Buf()]
        b_lg = [Buf(), Buf()]; b_ex = [Buf(), Buf()]; b_se = [Buf(), Buf()]
        b_po = [Buf(), Buf()]; b_pT = Buf(); b_plg = [Buf(), Buf()]; b_paT = [Buf(), Buf()]
        wsem3 = p.new_dsem()
        ld3 = [p.new_dsem(), p.new_dsem()]
        ld_ut = [p.new_dsem(), p.new_dsem()]
        ld_cbt = [p.new_dsem(), p.new_dsem()]
        st_h1 = [p.new_dsem() for _ in range(4)]
        st_hs = [p.new_dsem(), p.new_dsem()]
        xsem3 = [p.new_dsem(), p.new_dsem()]

        woutv = wout_d.rearrange("(c p) n -> p c n", p=128)
        for c in range(8):
            p.dma("pool", I_dma(wout_sb[:, c, :], woutv[:, c, :]), wsem3)
        b_wout.w = [(wsem3, wsem3.total)]
        p.load1(wr_sb, wr_d.rearrange("(c p) e -> p c e", p=128), b_wr)
        b_wrh = Buf()
        p.op("dve", I_copy(wr_hi, wr_sb), reads=[b_wr], writes=[b_wrh])
        p.op("dve", I_tt(wr_lo, wr_sb, wr_hi, ALU.subtract), reads=[b_wr, b_wrh], writes=[])
        b_wr.w = [("dve", p.cnt["dve"])]
        p.load1(g2b, g2_d, b_g2)
        p.load1(tokid, tokid_d, b_tok)
        p.op("dve", I_copy(comb[:, :, :, 0], tokid.unsqueeze(2).to_broadcast([128, NT, NE])), reads=[b_tok],
             writes=[b_comb])
        p.op("pool", I_memset(mixT[0], 0.0), writes=[b_mix[0]])
        p.op("pool", I_memset(mixT[1], 0.0), writes=[b_mix[1]])

        def sup(T):
            nsub = 4 if T < 16 else 1
            return nsub, nsub * 128, T * 512, (nsub * 128 if T < 16 else NMETA)

        def load3(T):
            nsub, N, n0, NV = sup(T)
            sl = T % 2
            p.dma("sp", I_dma(mixT[sl][:, 0:4, 0:N], attnT_d[:, :, n0:n0 + N].rearrange("h p n -> p h n")), ld3[sl],
                  writes=[b_mix[sl]])
            load_x(T, None, xt3, xsem3, b_xt3)
            p.dma("sp", I_dma(mixT[sl][:, 4:8, 0:N], convT_d[:, :, n0:n0 + N].rearrange("j p n -> p j n")), ld3[sl])
            b_mix[sl].w.append((ld3[sl], ld3[sl].total))

        subs = []
        for T in range(NSUP):
            for s in range(4 if T < 16 else 1):
                subs.append((T, s))
        NJ = len(subs)

        def S0(j):
            T, s = subs[j]
            sl = T % 2
            MT = mixT[sl]
            po = bank(2 * (j % 2), 2)
            lst = []
            for half in range(2):
                for m in range(8):
                    lst.append((po[:, half * 512:(half + 1) * 512], MT[:, m, s * 128:(s + 1) * 128],
                                wout_sb[:, m, half * 512:(half + 1) * 512], m == 0, m == 7, {}))
            p.op("pe", I_mm(lst), reads=[b_mix[sl], b_wout] + b_mixc[sl], writes=[b_po[j % 2]])

        def S1(j):
            T, s = subs[j]
            sl = T % 2
            hb = j % 2
            r0 = T * 512 + s * 128
            h4 = j % 4
            p.op("dve", I_tt(h1[h4], bank(2 * hb, 2), xt3[sl][:, s, :], ALU.add), reads=[b_po[hb], b_xt3[sl]],
                 writes=[b_h1[h4]])
            p.dma("pool", I_dma(hacc_d[r0:r0 + 128, :], h1[h4]), st_h1[h4], reads=[b_h1[h4]])
            if h1dbg_d is not None:
                p.dma("pool", I_dma(h1dbg_d[r0:r0 + 128, :], h1[h4]), st_h1[h4], reads=[b_h1[h4]])
            p.op("act", I_act(sqjunk, h1[h4], AF.Square, accum_out=c3[:, hb:hb + 1]), reads=[b_h1[h4]],
                 writes=[b_sqj, b_ss3[hb]])

        def S2(j):
            hb = j % 2
            rstd_chain(c3[:, hb:hb + 1], float(D), c3[:, 2 + hb:3 + hb], c3[:, 4 + hb:5 + hb], b_ss3[hb], b_t3[hb],
                       b_r3[hb])

        def S3a(j):
            T, s = subs[j]
            hb = j % 2
            r0 = T * 512 + s * 128
            p.op("dve", I_stt(hsf[hb], h1[j % 4], c3[:, 4 + hb:5 + hb], g2b, ALU.mult, ALU.mult),
                 reads=[b_h1[j % 4], b_r3[hb], b_g2], writes=[b_hsf[hb]])
            p.op("act", I_acopy(hhi[hb], hsf[hb]), reads=[b_hsf[hb]], writes=[b_hhi[hb]])
            p.dma("pool", I_dma(hs_d[r0:r0 + 128, :], hhi[hb]), st_hs[hb], reads=[b_hhi[hb]])

        def S3b(j):
            hb = j % 2
            p.op("dve", I_tt(hlo[hb], hsf[hb], hhi[hb], ALU.subtract), reads=[b_hsf[hb], b_hhi[hb]],
                 writes=[b_hlo[hb]])

        def S4(j):
            hb = j % 2
            pT = bank(4, 2).bitcast(BF16).rearrange("p (c t) -> p c t", t=128)
            lst = [(pT[:, c, :], hhi[hb][:, c * 128:(c + 1) * 128], ident_b) for c in range(8)]
            lst += [(pT[:, 8 + c, :], hlo[hb][:, c * 128:(c + 1) * 128], ident_b) for c in range(8)]
            p.op("pe", I_trs(lst), reads=[b_hhi[hb], b_hlo[hb], b_ident], writes=[b_pT])
            p.op("act", I_acopy(hlT[hb], pT), reads=[b_pT], writes=[b_hlT[hb]])

        def S5a(j):
            hb = j % 2
            plg = bank(6)[:, hb * 16:(hb + 1) * 16]
            lst = []
            for c in range(8):
                lst.append((plg, hlT[hb][:, c, :], wr_hi[:, c, :], c == 0, False, {}))
                lst.append((plg, hlT[hb][:, c, :], wr_lo[:, c, :], False, False, {}))
                lst.append((plg, hlT[hb][:, 8 + c, :], wr_hi[:, c, :], False, c == 7, {}))
            p.op("pe", I_mm(lst), reads=[b_hlT[hb], b_wr], writes=[b_plg[hb]])
            p.op("dve", lambda e, plg=plg, hb=hb: e.reduce_max(out=c3[:, 8 + hb:9 + hb], in_=plg,
                                                               axis=mybir.AxisListType.X),
                 reads=[b_plg[hb]], writes=[b_lg[hb]])
            p.op("dve", I_ts(c3[:, 10 + hb:11 + hb], c3[:, 8 + hb:9 + hb], -1.0, None, ALU.mult), reads=[b_lg[hb]],
                 writes=[b_lg[hb]])
            p.op("act", I_act(ex[hb], plg, AF.Exp, bias=c3[:, 10 + hb:11 + hb], accum_out=c3[:, 12 + hb:13 + hb]),
                 reads=[b_plg[hb], b_lg[hb]], writes=[b_ex[hb], b_se[hb]])

        def S5b(j):
            hb = j % 2
            p.op("dve", I_recip(c3[:, 14 + hb:15 + hb], c3[:, 12 + hb:13 + hb]), reads=[b_se[hb]], writes=[b_se[hb]])
            p.op("dve", I_ts(comb[:, j, :, 1], ex[hb], c3[:, 14 + hb:15 + hb], None, ALU.mult),
                 reads=[b_ex[hb], b_se[hb]], writes=[b_comb])

        def S6(j):
            hb = j % 2
            paT = bank(7)[0:16, hb * 128:(hb + 1) * 128]
            p.op("pe", I_trs([(paT, comb[:, j, :, 1], ident_f)]), reads=[b_comb, b_ident], writes=[b_paT[hb]])
            p.op("dve", I_copy(affT[0:16, j * 128:(j + 1) * 128], paT), reads=[b_paT[hb]], writes=[])

        stages = [S0, S1, S2, S3a, S3b, S4, S5a, S5b, S6]
        load3(0)
        for t in range(NJ + len(stages) - 1):
            for k in reversed(range(len(stages))):
                j = t - k
                if 0 <= j < NJ:
                    stages[k](j)
            if t < NJ:
                T, s = subs[t]
                if s == 0 and T + 1 < NSUP:
                    load3(T + 1)
        b_affT.w = [("dve", p.cnt["dve"])]
        p.barrier()
        ar.top = p34_top

        ar.top = p34_top
        wslot = [ar.alloc_top([8, 1024], BF16) for _ in range(6)]
        b_ws = [Buf() for _ in range(6)]
        wsem5 = [p.new_dsem() for _ in range(6)]

        def load_w(e_, k):
            if k < 4:
                src = (wg_d if k % 2 == 0 else wu_d)[e_].rearrange("(c p) f -> p c f", p=128)
                fh = k // 2
                src = src[:, :, fh * 1024:(fh + 1) * 1024]
            else:
                fh = k - 4
                src = wd_d[e_].rearrange("(c p) d -> p c d", p=128)[:, fh * 8:(fh + 1) * 8, :]
            for c in range(0, 8, 2):
                p.dma("pool", I_dma(wslot[k][:, c:c + 2, :], src[:, c:c + 2, :]), wsem5[k],
                      writes=[b_ws[k]] if c == 0 else [])
            b_ws[k].w = [(wsem5[k], wsem5[k].total)]

        for k in range(6):
            load_w(0, k)

        SEG = L // 8
        A128 = ar.alloc([SEG])
        msk = ar.alloc([SEG])
        cum = ar.alloc([SEG])
        ones = ar.alloc([SEG])
        gmat = ar.alloc([128])
        tmat = ar.alloc([128])
        c4 = ar.alloc([16])
        slotf = affT
        b4 = Buf()
        b_A = Buf(); b_gm = Buf(); b_tm = Buf(); b_cnt = Buf(); b_tot = Buf(); b_sl = Buf(); b_si = Buf()
        b_p4 = Buf(); b_s128 = Buf()
        bsem = [p.new_dsem() for _ in range(4)]
        p.load1(gmat, gmat_d, b_gm)
        p.load1(tmat, tmat_d, b_tm)
        t_st = p.dma("sp", I_dma(aff_d, affT[0:16, 0:L]), bsem[0], reads=[b_affT])
        p.dma("sp", I_dma(A128, aff_d.rearrange("e (s c) -> (e s) c", c=SEG)), bsem[1], writes=[b_A], extra=[t_st])
        lo = c4[:, 0:1]
        mid = c4[:, 1:2]
        cnt = c4[:, 2:3]
        dl = c4[:, 3:4]
        tot_ps = bank(3)[:, 0:1]
        p.op("dve", I_memset(c4, 0.0), writes=[b4])
        p.op("dve", I_memset(ones, 1.0), writes=[b4])
        junk3 = ar.alloc([SEG])
        w3 = c4[:, 5:8]
        tvec = c4[:, 8:11]
        cnt3 = c4[:, 11:14]
        ssum = c4[:, 14:15]
        ge3 = ar.alloc([8])
        tot3_ps = bank(3)[:, 0:3]
        b_tv = Buf(); b_c3 = Buf(); b_s = Buf(); b_lo = Buf()
        for i_ in range(3):
            p.op("dve", I_memset(c4[:, 5 + i_:6 + i_], float(i_ + 1)), writes=[b4])
        b_lo.w = list(b4.w)
        jbufs = [msk, cum, junk3]
        for k in range(NBIS // 2):
            w = 4.0 ** -(k + 1)
            p.op("dve", I_ts(tvec, w3, w, lo, ALU.mult, ALU.add), reads=[b_lo], writes=[b_tv])
            toks = []
            for i_ in range(3):
                toks.append(p.op("dve", I_ts(jbufs[i_], A128, tvec[:, i_:i_ + 1], 0.0, ALU.is_ge, ALU.add,
                                             accum=cnt3[:, i_:i_ + 1]),
                                 reads=[b_A, b_tv], writes=[b_c3] if i_ == 0 else []))
            b_c3.w = toks
            p.op("pe", I_mm([(tot3_ps, gmat, cnt3, True, True, {})]), reads=[b_c3, b_gm], writes=[b_tot])
            p.op("dve", I_ts(ge3[:, 0:3], tot3_ps, float(CAP) - 0.5, 0.0, ALU.is_ge, ALU.add, accum=ssum),
                 reads=[b_tot], writes=[b_s])
            p.op("dve", I_stt(lo, ssum, w, lo, ALU.mult, ALU.add), reads=[b_s], writes=[b_lo])
        b4.w = list(b_lo.w) + list(b_c3.w)
        p.op("dve", I_ts(msk, A128, lo, None, ALU.is_ge), reads=[b_A], writes=[b4])
        p.op("dve", lambda e: e.tensor_tensor_scan(out=cum, data0=ones, data1=msk, initial=0.0, op0=ALU.mult,
                                                   op1=ALU.add), writes=[b4, b_cnt])
        p.op("pe", I_mm([(tot_ps, tmat, cum[:, SEG - 1:SEG], True, True, {})]), reads=[b_cnt, b_tm], writes=[b_tot])
        p.op("dve", I_ts(c4[:, 4:5], tot_ps, -(BIG + 1.0), None, ALU.add), reads=[b_tot], writes=[b4])
        p.op("dve", I_stt(cum, cum, c4[:, 4:5], msk, ALU.add, ALU.mult), writes=[b4])
        p.op("dve", I_ts(cum, cum, BIG, None, ALU.add), writes=[b4, b_s128])
        t_st2 = p.dma("sp", I_dma(aff_d.rearrange("e (s c) -> (e s) c", c=SEG), cum), bsem[2], reads=[b_s128],
                      extra=[(bsem[1], bsem[1].total)])
        p.op("dve", I_memset(slotf[0:16, L:LP], BIG), writes=[b4])
        t_ld2 = p.dma("sp", I_dma(slotf[0:16, 0:L], aff_d), bsem[3], extra=[t_st2, (bsem[0], bsem[0].total)])
        b_sl.w = [t_ld2, ("dve", p.cnt["dve"])]
        for g in range(3):
            j0 = g * 32
            j1 = min(NT, j0 + 32)
            pg = bank(g)
            p.op("pe", I_trs([(pg[:, (j - j0) * 16:(j - j0 + 1) * 16], slotf[0:16, j * 128:(j + 1) * 128],
                               ident_f[0:16, 0:16]) for j in range(j0, j1)]),
                 reads=[b_sl, b_ident], writes=[b_p4])
            p.op("dve", I_copy(slot_i[:, j0:j1, :], pg[:, 0:(j1 - j0) * 16].rearrange("p (j e) -> p j e", e=16)),
                 reads=[b_p4], writes=[b_si])
        csem = [p.new_dsem() for _ in range(NE)]

        def emit_comb_scatter(e_, j0=0, j1=NT):
            for j in range(j0, j1):
                def sc(e, e_=e_, j=j):
                    return e.indirect_dma_start(
                        out=comb_d[e_], out_offset=bass.IndirectOffsetOnAxis(ap=slot_i[:, j, e_:e_ + 1], axis=0),
                        in_=comb[:, j, e_, :], in_offset=None, bounds_check=bc_reg(e, CAPP - 1), oob_is_err=False)
                p.dma("pool", sc, csem[e_], reads=[b_si, b_comb])

        emit_comb_scatter(0)
        p.barrier_on([("dve", p.cnt["dve"]), ("pe", p.cnt["pe"]), ("act", p.cnt["act"])])
        ar.top = persist_top

        xg = ar.alloc([NCT, 1024], BF16)
        _xgT = ar.alloc([8, CAPP], BF16)
        xgT = [_xgT, _xgT]
        actT = ar.alloc([16, CAPP], BF16)
        sil = [ar.alloc([512]) for _ in range(2)]
        NY = 4
        y_st = [ar.alloc([1024]) for _ in range(NY)]
        cg = [ar.alloc([NCT, 2]) for _ in range(2)]
        idx_i = [ar.alloc([NCT], I32) for _ in range(2)]
        b_xg = [Buf() for _ in range(NCT)]
        _bx = [Buf() for _ in range(NCT)]
        b_xgT = [_bx, _bx]
        b_act = [Buf() for _ in range(16)]
        b_sil = [Buf(), Buf()]
        b_y = [Buf() for _ in range(4)]
        b_cg = [Buf(), Buf()]
        b_idx = [Buf(), Buf()]
        b_p5 = [Buf() for _ in range(8)]
        cgsem = [p.new_dsem(), p.new_dsem()]
        gsem = [p.new_dsem() for _ in range(NCT)]
        scsem = [p.new_dsem() for _ in range(4)]

        def emit_gather(e_):
            sl = e_ % 2
            p.dma("sp", I_dma(cg[sl], comb_d[e_].rearrange("(ct p) t -> p ct t", p=128)), cgsem[sl],
                  writes=[b_cg[sl]], extra=[(csem[e_], csem[e_].total)])
            p.op("dve", I_copy(idx_i[sl], cg[sl][:, :, 0]), reads=[b_cg[sl]], writes=[b_idx[sl]])
            for ct in range(NCT):
                def ga(e, sl=sl, ct=ct):
                    return e.indirect_dma_start(
                        out=xg[:, ct, :], out_offset=None, in_=hs_d,
                        in_offset=bass.IndirectOffsetOnAxis(ap=idx_i[sl][:, ct:ct + 1], axis=0),
                        bounds_check=bc_reg(e, ROWS - 1), oob_is_err=True)
                p.dma("pool", ga, gsem[ct], reads=[b_idx[sl]], writes=[b_xg[ct]])

        p.op("dve", I_memset(actT, 0.0), writes=b_act)
        tr_cnt = [0]

        def emit_xg_transposes(e_):
            sl = e_ % 2
            for ct in range(NCT):
                bk = 6 + tr_cnt[0] % 2
                tr_cnt[0] += 1
                pb = bank(bk).bitcast(BF16).rearrange("p (c t) -> p c t", t=128)
                p.op("pe", I_trs([(pb[:, c, :], xg[:, ct, c * 128:(c + 1) * 128], ident_b) for c in range(8)]),
                     reads=[b_xg[ct], b_ident], writes=[b_p5[bk]])
                p.op("act" if ct % 2 == 0 else "dve",
                     (I_acopy if ct % 2 == 0 else I_copy)(xgT[sl][:, :, ct * 128:(ct + 1) * 128], pb),
                     reads=[b_p5[bk]], writes=[b_xgT[sl][ct]])

        CG = [(0, 342), (342, 342), (684, 342)]
        gu_cnt = [0]

        def emit_gateup(e_, fh):
            sl = e_ % 2
            XT = xgT[sl]
            for fc in range(8):
                f = fh * 8 + fc
                for gi, (c0, n) in enumerate(CG):
                    ab = gu_cnt[0] % 2
                    gu_cnt[0] += 1
                    pA = bank(2 * ab)
                    pB = bank(2 * ab + 1)
                    rx = [b_xgT[sl][ct] for ct in range(c0 // 128, (c0 + n + 127) // 128)]
                    p.op("pe", I_mm([(pA[:, 0:n], wslot[fh * 2][:, c, fc * 128:(fc + 1) * 128], XT[:, c, c0:c0 + n],
                                      c == 0, c == 7, {}) for c in range(8)]),
                         reads=rx + [b_ws[fh * 2]], writes=[b_p5[2 * ab]])
                    p.op("pe", I_mm([(pB[:, 0:n], wslot[fh * 2 + 1][:, c, fc * 128:(fc + 1) * 128],
                                      XT[:, c, c0:c0 + n], c == 0, c == 7, {}) for c in range(8)]),
                         reads=rx + [b_ws[fh * 2 + 1]], writes=[b_p5[2 * ab + 1]])
                    p.op("act", I_act(sil[ab][:, 0:n], pA[:, 0:n], AF.Silu), reads=[b_p5[2 * ab]], writes=[b_sil[ab]])
                    tok = p.op("dve", I_tt(actT[:, f, c0:c0 + n], sil[ab][:, 0:n], pB[:, 0:n], ALU.mult),
                               reads=[b_sil[ab], b_p5[2 * ab + 1]], extra=list(b_act[f].r) if gi == 0 else [])
                    if gi == 2:
                        b_act[f].w = [tok]
                        b_act[f].r = []

        y_cnt = [0]

        def emit_down(e_, ct):
            sl = e_ % 2
            ys = y_cnt[0] % NY
            y_cnt[0] += 1
            for half in range(2):
                pY = bank(4 + half)
                p.op("pe", I_mm([(pY, actT[:, f, ct * 128:(ct + 1) * 128],
                                  wslot[4 + f // 8][:, f % 8, half * 512:(half + 1) * 512], f == 0, f == 15, {})
                                 for f in range(16)]),
                     reads=b_act + [b_ws[4], b_ws[5]], writes=[b_p5[4 + half]])
                p.op("dve", I_ts(y_st[ys][:, half * 512:(half + 1) * 512], pY, cg[sl][:, ct, 1:2], None, ALU.mult),
                     reads=[b_p5[4 + half], b_cg[sl]], writes=[b_y[ys]] if half == 0 else [])
            b_y[ys].w = [("dve", p.cnt["dve"])]

            def sa(e, sl=sl, ct=ct, ys=ys):
                return e.indirect_dma_start(
                    out=hacc_d, out_offset=bass.IndirectOffsetOnAxis(ap=idx_i[sl][:, ct:ct + 1], axis=0),
                    in_=y_st[ys], in_offset=None, bounds_check=bc_reg(e, ROWS - 1), oob_is_err=True, compute_op=ALU.add)
            extra = list(prev_sc[0]) if ct == 0 else []
            p.dma("pool", sa, scsem[ys], reads=[b_y[ys], b_idx[sl]], extra=extra)
            if ct == NCT - 1:
                prev_sc[0] = [(sc_, sc_.total) for sc_ in scsem if sc_.total > 0]

        prev_sc = [[]]
        emit_gather(0)
        emit_comb_scatter(1)
        emit_xg_transposes(0)
        for e_ in range(NE):
            if e_ + 2 < NE and e_ > 0:
                emit_comb_scatter(e_ + 2, 0, 36)
            emit_gateup(e_, 0)
            if e_ + 1 < NE:
                load_w(e_ + 1, 0)
                load_w(e_ + 1, 1)
            if e_ + 2 < NE and e_ > 0:
                emit_comb_scatter(e_ + 2, 36, NT)
            if e_ + 1 < NE and e_ > 0:
                emit_gather(e_ + 1)
            emit_gateup(e_, 1)
            if e_ + 1 < NE:
                load_w(e_ + 1, 2)
                load_w(e_ + 1, 3)
                if e_ == 0:
                    emit_gather(e_ + 1)
            for ct in range(NCT):
                emit_down(e_, ct)
                if ct == 3 and e_ + 1 < NE:
                    emit_xg_transposes(e_ + 1)
            if e_ + 1 < NE:
                load_w(e_ + 1, 4)
                load_w(e_ + 1, 5)
            if e_ == 0:
                emit_comb_scatter(2)
        p.barrier()
        ar.top = persist_top

        ar.n = ARENA_WORDS
        g3b = ar.alloc([1024])
        hx = [ar.alloc([4, 1024]) for _ in range(3)]
        ox = [ar.alloc([4, 1024]) for _ in range(3)]
        jk6 = ar.alloc([1024], BF16)
        c6 = ar.alloc([16])
        b_g3 = Buf(); b_hx = [Buf() for _ in range(3)]; b_ox = [Buf() for _ in range(3)]; b_j6 = Buf(); b_s6 = Buf(); b_t6 = Buf(); b_r6 = Buf()
        l6 = [p.new_dsem() for _ in range(3)]
        s6 = [p.new_dsem() for _ in range(3)]
        p.load1(g3b, g3_d, b_g3)

        def load6(T):
            p.dma("sp", I_dma(hx[T % 3], hacc_d[T * 512:(T + 1) * 512, :].rearrange("(s p) d -> p s d", p=128)),
                  l6[T % 3], writes=[b_hx[T % 3]])

        load6(0)
        load6(1)
        for T in range(16):
            if T + 2 < 16:
                load6(T + 2)
            X = hx[T % 3]
            for s in range(4):
                p.op("act", I_act(jk6, X[:, s, :], AF.Square, accum_out=c6[:, s:s + 1]), reads=[b_hx[T % 3]],
                     writes=[b_j6, b_s6] if s == 0 else [b_j6])
            b_s6.w = [("act", p.cnt["act"])]
            rstd_chain(c6[:, 0:4], float(D), c6[:, 4:8], c6[:, 8:12], b_s6, b_t6, b_r6)
            for s in range(4):
                p.op("dve", I_stt(ox[T % 3][:, s, :], X[:, s, :], c6[:, 8 + s:9 + s], g3b, ALU.mult, ALU.mult),
                     reads=[b_hx[T % 3], b_r6, b_g3], writes=[b_ox[T % 3]] if s == 0 else [])
            b_ox[T % 3].w = [("dve", p.cnt["dve"])]
            p.dma("pool", I_dma(out_d[T * 512:(T + 1) * 512, :].rearrange("(s p) d -> p s d", p=128), ox[T % 3]),
                  s6[T % 3], reads=[b_ox[T % 3]])
        p.barrier()

        with nc.Block() as block:
            @block.sync
            def _(e):
                p.replay("sp", e)

            @block.gpsimd
            def _(e):
                p.replay("pool", e)

            @block.scalar
            def _(e):
                p.replay("act", e)

            @block.vector
            def _(e):
                p.replay("dve", e)

            @block.tensor
            def _(e):
                p.replay("pe", e)
    return nc


_NC_CACHE = {}


def _rope_table(scale):
    n = np.arange(LP)
    pos = np.where(n < SEQ, n + NMETA, n - SEQ)
    pos = np.where(n < L, pos, 0).astype(np.float32)
    inv_freq = (np.float32(10000.0) ** (-(np.arange(0, 64, 2, dtype=np.float32)) / np.float32(64))).astype(np.float32)
    ang = (pos[:, None] * inv_freq[None, :]).astype(np.float32)
    cos = np.cos(ang).astype(np.float32)
    sin = np.sin(ang).astype(np.float32)
    t = np.concatenate([cos, cos, -sin, sin], axis=1).astype(np.float32) * np.float32(scale)
    return np.ascontiguousarray(t)


def kernel(x, meta_tokens, mix_norm_g, w_in, conv_w, lambda_q1, lambda_k1, lambda_q2, lambda_k2, attn_subln_g,
           w_out, ffn_norm_g, w_router, w_gate, w_up, w_down, final_norm_g):
    f = lambda a: np.ascontiguousarray(np.asarray(a, dtype=np.float32))
    x = f(x)
    B = x.shape[0]
    if "nc" not in _NC_CACHE:
        _NC_CACHE["nc"] = build_nc()
    nc = _NC_CACHE["nc"]
    tile128 = lambda v: np.ascontiguousarray(np.broadcast_to(f(v).reshape(1, -1), (128, f(v).size)))
    convw = np.ascontiguousarray(f(conv_w)[0].reshape(3, 4, 128).transpose(2, 1, 0))
    lamp = np.ascontiguousarray(np.broadcast_to(
        np.stack([f(lambda_q1)[0], f(lambda_k1)[0], f(lambda_q2)[0], f(lambda_k2)[0]])[None], (128, 4, 64)))
    tokid = np.ascontiguousarray((np.arange(NT)[None, :] * 128 + np.arange(128)[:, None]).astype(np.float32))
    pp = np.arange(128)
    same = (pp[:, None] // 8) == (pp[None, :] // 8)
    gmat = np.ascontiguousarray(same.astype(np.float32))
    tmat = np.ascontiguousarray((same & (pp[:, None] < pp[None, :])).astype(np.float32))
    dummy = np.zeros((128, NCT, 2), np.float32)
    dummy[:, :, 0] = (L + np.arange(128))[:, None]
    shared = {
        "meta": f(meta_tokens),
        "g1b": tile128(f(mix_norm_g)[0]), "g2b": tile128(f(ffn_norm_g)[0]), "g3b": tile128(f(final_norm_g)),
        "w_in": f(w_in)[0], "w_out": f(w_out)[0], "w_router": f(w_router)[0],
        "w_gate": f(w_gate)[0], "w_up": f(w_up)[0], "w_down": f(w_down)[0],
        "convw": convw, "lamp": lamp, "gsub": tile128(f(attn_subln_g)[0]),
        "ropeq": _rope_table(0.125), "ropek": _rope_table(1.0),
        "ident": np.eye(128, dtype=np.float32), "tokid": tokid, "dummyrows": dummy,
        "gmat": gmat, "tmat": tmat,
    }
    in_maps = []
    for b in range(B):
        m = dict(shared)
        m["x"] = x[b]
        in_maps.append(m)
    res = run_bass_kernel_spmd(nc, in_maps, core_ids=list(range(B)))
    if DEBUG:
        _NC_CACHE["dbg"] = {k: np.asarray(res.results[0][k]) for k in DEBUG}
    return np.stack([np.asarray(r["out"], dtype=np.float32) for r in res.results], axis=0)
```

```python
import contextlib
import math
import numpy as np
import concourse.bass as bass
import concourse.mybir as mybir
from concourse.bass_utils import run_bass_kernel_spmd

F32 = mybir.dt.float32
BF16 = mybir.dt.bfloat16
I32 = mybir.dt.int32
ALU = mybir.AluOpType
AF = mybir.ActivationFunctionType

D = 1024
SEQ = 8192
NMETA = 16
L = SEQ + NMETA
LP = 8320
NT = 65
NE = 16
CAP = 1026
CAPP = 1152
NCT = 9
DFF = 2048
ROWS = L + 128
EPS = 1e-6
LAM_INIT = 0.8 - 0.6 * math.exp(-0.3 * 0)
BIG = 5000.0
NBIS = 28
ARENA_WORDS = 53100


def _size(dt):
    return 2 if dt == BF16 else 4


class DSem:
    def __init__(self, h):
        self.h = h
        self.total = 0


class Buf:
    __slots__ = ("w", "r")

    def __init__(self):
        self.w = []
        self.r = []


class Prog:
    ENG = ("pe", "act", "dve", "pool", "sp")

    def __init__(self, nc, es):
        self.nc = nc
        self.es = es
        self.q = {k: [] for k in self.ENG}
        self.cnt = {k: 0 for k in ("pe", "act", "dve", "pool")}
        self.sem = {k: es.enter_context(nc.semaphore("s_" + k)) for k in self.cnt}
        self.dsems = []

    def new_dsem(self):
        h = self.es.enter_context(self.nc.semaphore("d%d" % len(self.dsems)))
        d = DSem(h)
        self.dsems.append(d)
        return d

    @staticmethod
    def _deps(reads, writes, extra):
        deps = list(extra)
        for b in reads:
            deps += b.w
        for b in writes:
            deps += b.w
            deps += b.r
        return deps

    @staticmethod
    def _commit(tok, reads, writes):
        for b in writes:
            b.w = [tok]
            b.r = []
        for b in reads:
            b.r = [t for t in b.r if t[0] != tok[0]] + [tok]

    def op(self, eng, fn, reads=(), writes=(), extra=()):
        deps = self._deps(reads, writes, extra)
        self.cnt[eng] += 1
        tok = (eng, self.cnt[eng])
        self.q[eng].append((fn, deps, tok, 1))
        self._commit(tok, reads, writes)
        return tok

    def dma(self, queue, fn, dsem, reads=(), writes=(), extra=()):
        deps = self._deps(reads, writes, extra)
        dsem.total += 16
        tok = (dsem, dsem.total)
        self.q[queue].append((fn, deps, tok, 16))
        self._commit(tok, reads, writes)
        return tok

    def barrier(self):
        toks = [(k, self.cnt[k]) for k in self.cnt if self.cnt[k] > 0]
        toks += [(d, d.total) for d in self.dsems if d.total > 0]
        for k in self.ENG:
            self.q[k].append((None, toks, None, 0))

    def barrier_on(self, toks):
        for k in self.ENG:
            self.q[k].append((None, list(toks), None, 0))

    def load1(self, out, in_, b):
        return self.dma("sp", I_dma(out, in_), self.new_dsem(), writes=[b])

    def _h(self, k):
        return self.sem[k] if isinstance(k, str) else k.h

    def replay(self, name, e):
        waited = {}
        for fn, deps, tok, inc in self.q[name]:
            need = {}
            for k, v in deps:
                if name == "pe" and k == "pe" and fn is not None:
                    continue
                if v > need.get(k, 0):
                    need[k] = v
            for k, v in need.items():
                if waited.get(k, 0) < v:
                    e.wait_ge(self._h(k), v)
                    waited[k] = v
            if fn is None:
                continue
            ins = fn(e)
            ins.then_inc(self._h(tok[0]), inc)


class Arena:
    def __init__(self, t, nwords):
        self.t = t
        self.n = nwords
        self.top = 0

    def alloc_top(self, shape, dt=F32):
        nel = int(np.prod(shape))
        nw = ((nel * _size(dt) + 3) // 4 + 7) // 8 * 8
        self.n -= nw
        save = self.top
        self.top = self.n
        self.n += nw
        a = self.alloc(shape, dt)
        self.n -= nw
        self.top = save
        assert self.top <= self.n
        return a

    def alloc(self, shape, dt=F32):
        nel = int(np.prod(shape))
        nw = (nel * _size(dt) + 3) // 4
        nw = (nw + 7) // 8 * 8
        assert self.top + nw <= self.n, ("arena overflow", self.top, nw)
        a = self.t[:, self.top:self.top + nw]
        self.top += nw
        if dt != F32:
            a = a.bitcast(dt)
        a = a[:, 0:nel]
        if len(shape) == 2:
            a = a.rearrange("p (a b) -> p a b", b=shape[1])
        elif len(shape) == 3:
            a = a.rearrange("p (a b c) -> p a b c", b=shape[1], c=shape[2])
        return a


def I_dma(out, in_):
    return lambda e: e.dma_start(out=out, in_=in_)


def I_dma_nc(out, in_):
    return lambda e: e.dma_start(out=out, in_=in_, allow_slow_non_contiguous=True)


_REGS = {}


def bc_reg(e, v):
    if v not in _REGS:
        _REGS[v] = e.to_reg(v)
    return _REGS[v]


def I_act(out, in_, func, **kw):
    return lambda e: e.activation(out=out, in_=in_, func=func, **kw)


def I_tt(out, a, b, op):
    return lambda e: e.tensor_tensor(out=out, in0=a, in1=b, op=op)


def I_ts(out, a, s1, s2, op0, op1=None, accum=None):
    def f(e):
        kw = {}
        if op1 is not None:
            kw["op1"] = op1
        if accum is not None:
            kw["accum_out"] = accum
        return e.tensor_scalar(out=out, in0=a, scalar1=s1, scalar2=s2, op0=op0, **kw)
    return f


def I_stt(out, a, sc, b, op0, op1, accum=None):
    def f(e):
        kw = {}
        if accum is not None:
            kw["accum_out"] = accum
        return e.scalar_tensor_tensor(out=out, in0=a, scalar=sc, in1=b, op0=op0, op1=op1, **kw)
    return f


def I_copy(out, in_):
    return lambda e: e.tensor_copy(out=out, in_=in_)


def I_acopy(out, in_):
    return lambda e: e.copy(out=out, in_=in_)


def I_memset(ap, v):
    return lambda e: e.memset(ap, v)


def I_recip(out, in_):
    return lambda e: e.reciprocal(out=out, in_=in_)


def I_mm(lst):
    def f(e):
        ins = None
        for (out, lhsT, rhs, st, sp, kw) in lst:
            ins = e.matmul(out, lhsT=lhsT, rhs=rhs, start=st, stop=sp, **kw)
        return ins
    return f


def I_trs(lst):
    def f(e):
        ins = None
        for (out, in_, ident) in lst:
            ins = e.transpose(out=out, in_=in_, identity=ident)
        return ins
    return f


DEBUG = []


def build_nc():
    _REGS.clear()
    nc = bass.Bass("TRN2", target_bir_lowering=False)

    def din(name, shape, dt=F32):
        return nc.dram_tensor(name, shape, dt, kind="ExternalInput").ap()

    def dint(name, shape, dt):
        kind = "ExternalOutput" if name in DEBUG else "Internal"
        return nc.dram_tensor(name, shape, dt, kind=kind).ap()

    x_d = din("x", [SEQ, D])
    meta_d = din("meta", [NMETA, D])
    g1_d = din("g1b", [128, D])
    g2_d = din("g2b", [128, D])
    g3_d = din("g3b", [128, D])
    win_d = din("w_in", [D, 3072])
    wout_d = din("w_out", [D, D])
    wr_d = din("w_router", [D, NE])
    wg_d = din("w_gate", [NE, D, DFF])
    wu_d = din("w_up", [NE, D, DFF])
    wd_d = din("w_down", [NE, DFF, D])
    convw_d = din("convw", [128, 4, 3])
    lamp_d = din("lamp", [128, 4, 64])
    gsub_d = din("gsub", [128, 128])
    ropeq_d = din("ropeq", [LP, 128])
    ropek_d = din("ropek", [LP, 128])
    ident_d = din("ident", [128, 128])
    tokid_d = din("tokid", [128, NT])
    dummy_d = din("dummyrows", [128, NCT, 2])
    gmat_d = din("gmat", [128, 128])
    tmat_d = din("tmat", [128, 128])
    out_d = nc.dram_tensor("out", [SEQ, D], F32, kind="ExternalOutput").ap()

    qT_d = dint("qT_s", [4, 128, LP], BF16)
    kT_d = dint("kT_s", [4, 128, LP], BF16)
    v_d = dint("v_s", [4, 128, NT, 128], BF16)
    uT_d = dint("uT_s", [4, 128, L + 2], F32)
    cbT_d = dint("cbT_s", [4, 128, LP], F32)
    attnT_d = dint("attnT_s", [4, 128, LP], BF16)
    convT_d = dint("convT_s", [4, 128, LP], BF16)
    aff_d = dint("aff_s", [NE, L], F32)
    hacc_d = dint("hacc_s", [ROWS, D], F32)
    hs_d = dint("hs_s", [ROWS, D], BF16)
    comb_d = [dint("comb_s%d" % e, [CAPP, 2], F32) for e in range(NE)]
    h1dbg_d = dint("h1dbg_s", [LP, D], F32) if "h1dbg_s" in DEBUG else None

    es = contextlib.ExitStack()
    with es:
        arena_t = es.enter_context(nc.sbuf_tensor("arena", [128, ARENA_WORDS], F32))
        ps = es.enter_context(nc.psum_tensor("ps", [128, 4096], F32))
        p = Prog(nc, es)
        ar = Arena(arena_t, ARENA_WORDS)

        def bank(b, n=1):
            return ps[:, b * 512:(b + n) * 512]

        ident_f = ar.alloc([128])
        ident_b = ar.alloc([128], BF16)
        cols = ar.alloc([64])
        lam_col = cols[:, 0:1]
        zt = ar.alloc([1024])
        misc_ld = p.new_dsem()
        misc_st = p.new_dsem()
        b_ident = Buf()
        b_lam = Buf()
        b_z = Buf()

        p.load1(ident_f, ident_d, b_ident)
        p.op("dve", I_copy(ident_b, ident_f), reads=[b_ident], writes=[b_ident])
        p.op("pool", I_memset(zt, 0.0), writes=[b_z])

        lamp = ar.alloc([4, 64])
        ljunk = ar.alloc([64])
        b_lp = Buf()
        p.load1(lamp, lamp_d, b_lp)
        p.op("dve", I_stt(ljunk, lamp[:, 0, :], 1.0, lamp[:, 1, :], ALU.mult, ALU.mult, accum=cols[:, 1:2]),
             reads=[b_lp], writes=[b_lam])
        p.op("dve", I_stt(ljunk, lamp[:, 2, :], 1.0, lamp[:, 3, :], ALU.mult, ALU.mult, accum=cols[:, 2:3]),
             reads=[b_lp, b_lam], writes=[b_lam])
        p.op("act", I_act(cols[:, 3:5], cols[:, 1:3], AF.Exp), reads=[b_lam], writes=[b_lam])
        p.op("dve", I_tt(cols[:, 5:6], cols[:, 3:4], cols[:, 4:5], ALU.subtract), reads=[b_lam], writes=[b_lam])
        p.op("dve", I_ts(lam_col, cols[:, 5:6], float(LAM_INIT), None, ALU.add), reads=[b_lam], writes=[b_lam])

        dm = ar.alloc([NCT, 2])
        comb = ar.alloc([NT, NE, 2])
        slot_i = ar.alloc([NT, NE], I32)
        b_dm = Buf()

        def emit_scratch_init():
            p.dma("sp", I_dma_nc(uT_d[:, :, 0:1].rearrange("j p o -> p j o"), zt[:, 0:4].unsqueeze(2)), misc_st,
                  reads=[b_z])
            p.dma("sp", I_dma_nc(uT_d[:, :, L + 1:L + 2].rearrange("j p o -> p j o"), zt[:, 0:4].unsqueeze(2)),
                  misc_st, reads=[b_z])
            p.dma("sp", I_dma(hs_d[LP:ROWS, :], zt[0:16, 0:512].bitcast(BF16)), misc_st, reads=[b_z])
            p.dma("sp", I_dma(hacc_d[LP:ROWS, :], zt[0:16, :]), misc_st, reads=[b_z])
            p.load1(dm, dummy_d, b_dm)
            for e_ in range(NE):
                p.dma("sp", I_dma(comb_d[e_].rearrange("(ct p) t -> p ct t", p=128), dm), misc_st, reads=[b_dm])

        persist_top = ar.top

        def rstd_chain(ss_ap, n, tmp_ap, out_ap, b_ss, b_tmp, b_out):
            p.op("dve", I_ts(tmp_ap, ss_ap, 1.0 / n, EPS, ALU.mult, ALU.add), reads=[b_ss], writes=[b_tmp])
            p.op("act", I_act(tmp_ap, tmp_ap, AF.Ln), reads=[], writes=[b_tmp])
            p.op("act", I_act(out_ap, tmp_ap, AF.Exp, scale=-0.5), reads=[b_tmp], writes=[b_out])

        win_sb = ar.alloc([8, 3072], BF16)
        g1b = ar.alloc([1024])
        b_win = Buf()
        b_g = Buf()
        wsem = p.new_dsem()
        winv = win_d.rearrange("(c p) n -> p c n", p=128)
        b_winf = Buf()
        wsemf = p.new_dsem()
        for c in range(8):
            p.dma("pool", I_dma(win_sb[:, c, 1536:3072], winv[:, c, 1536:3072]), wsemf)
        b_winf.w = [(wsemf, wsemf.total)]
        for c in range(8):
            p.dma("pool", I_dma(win_sb[:, c, 0:1536], winv[:, c, 0:1536]), wsem)
        b_win.w = [(wsem, wsem.total)]
        p.load1(g1b, g1_d, b_g)

        xt = [ar.alloc([4, 1024]) for _ in range(2)]
        rtq = ar.alloc([4, 128])
        rtk = ar.alloc([4, 128])
        c1 = ar.alloc([16])
        junkb = ar.alloc([1024], BF16)
        hn = ar.alloc([4, 1024], BF16)
        hnT = [ar.alloc([8, 512], BF16) for _ in range(2)]
        cx_sb = ar.alloc([512])
        uT_st = ar.alloc([4, 512])
        cbT_st = ar.alloc([4, 512])
        rt1 = ar.alloc([8, 64])
        rt2 = ar.alloc([8, 64])
        qk_tm = ar.alloc([4, 2, 512], BF16)
        v_st = ar.alloc([4, 512], BF16)
        qkT_st = ar.alloc([8, 512], BF16)

        b_xt = [Buf(), Buf()]
        b_rt = Buf()
        b_ss = Buf(); b_tmp = Buf(); b_rstd = Buf()
        b_junk = Buf()
        b_hn = [Buf() for _ in range(4)]
        b_hnT = [[Buf() for _ in range(4)] for _ in range(2)]
        b_cx = Buf(); b_uT = Buf(); b_cbT = Buf()
        b_r1 = Buf(); b_r2 = Buf()
        b_qktm = [[Buf() for _ in range(2)] for _ in range(4)]
        b_vst = Buf(); b_qkT = Buf()
        b_ps = [Buf() for _ in range(8)]
        xsem = [p.new_dsem(), p.new_dsem()]
        rsem = p.new_dsem()
        st_u = p.new_dsem(); st_cb = p.new_dsem(); st_qk = p.new_dsem(); st_v = p.new_dsem()

        def load_x(T, bufs, xtile, sems, bxt):
            nsub = 4 if T < 16 else 1
            n0 = T * 512
            if T < 16:
                p.dma("sp", I_dma(xtile[T % 2], x_d[n0:n0 + 512, :].rearrange("(s p) d -> p s d", p=128)),
                      sems[T % 2], writes=[bxt[T % 2]])
            else:
                tokm = p.op("pool", I_memset(xtile[T % 2][:, 0, :], 0.0), writes=[bxt[T % 2]])
                tokd = p.dma("sp", I_dma(xtile[T % 2][0:16, 0, :], meta_d), sems[T % 2], extra=[tokm])
                bxt[T % 2].w.append(tokd)

        NSUP = 17
        rtq2 = [rtq, ar.alloc([4, 128])]
        rtk2 = [rtk, ar.alloc([4, 128])]
        b_rt2 = [Buf(), Buf()]
        rsem2 = [p.new_dsem(), p.new_dsem()]

        def stageA(T):
            nsub = 4 if T < 16 else 1
            N = nsub * 128
            n0 = T * 512
            X = xt[T % 2]
            bX = b_xt[T % 2]
            HT = hnT[T % 2]
            bHT = b_hnT[T % 2]
            p.dma("sp", I_dma(rtq2[T % 2][:, 0:nsub, :], ropeq_d[n0:n0 + N, :].rearrange("(s p) f -> p s f", p=128)),
                  rsem2[T % 2], writes=[b_rt2[T % 2]])
            tk_ = p.dma("sp", I_dma(rtk2[T % 2][:, 0:nsub, :],
                                    ropek_d[n0:n0 + N, :].rearrange("(s p) f -> p s f", p=128)), rsem2[T % 2])
            b_rt2[T % 2].w.append(tk_)
            for s in range(nsub):
                p.op("act", I_act(junkb, X[:, s, :], AF.Square, accum_out=c1[:, s:s + 1]),
                     reads=[bX], writes=[b_junk, b_ss] if s == 0 else [b_junk])
            b_ss.w = [("act", p.cnt["act"])]
            rstd_chain(c1[:, 0:nsub], float(D), c1[:, 4:4 + nsub], c1[:, 8:8 + nsub], b_ss, b_tmp, b_rstd)
            for s in range(nsub):
                p.op("dve", I_stt(hn[:, s, :], X[:, s, :], c1[:, 8 + s:9 + s], g1b, ALU.mult, ALU.mult),
                     reads=[bX, b_rstd, b_g], writes=[b_hn[s]])
            for s in range(nsub):
                pb = bank(s % 2).bitcast(BF16).rearrange("p (c t) -> p c t", t=128)
                p.op("pe", I_trs([(pb[:, c, :], hn[:, s, c * 128:(c + 1) * 128], ident_b) for c in range(8)]),
                     reads=[b_hn[s], b_ident], writes=[b_ps[s % 2]])
                p.op("act" if s % 2 == 0 else "dve",
                     (I_acopy if s % 2 == 0 else I_copy)(HT[:, :, s * 128:(s + 1) * 128], pb),
                     reads=[b_ps[s % 2]], writes=[bHT[s]])

        def chunksB(T):
            nsub = 4 if T < 16 else 1
            N = nsub * 128
            n0 = T * 512
            HT = hnT[T % 2]
            bHT = b_hnT[T % 2]
            rHT = [bHT[s] for s in range(nsub)]
            rtqT = rtq2[T % 2]
            rtkT = rtk2[T % 2]
            bRT = b_rt2[T % 2]
            out = []
            fmc = [0]

            def fm_chunk(j):
                def fmm(col0, pbk):
                    return I_mm([(pbk[:, 0:N], win_sb[:, c, col0:col0 + 128], HT[:, c, 0:N], c == 0, c == 7, {})
                                 for c in range(8)])
                pcx = bank(2 + fmc[0] % 2); bcx = b_ps[2 + fmc[0] % 2]; fmc[0] += 1
                p.op("pe", fmm(1536 + j * 128, pcx), reads=rHT + [b_winf], writes=[bcx])
                p.op("act", I_acopy(cx_sb[:, 0:N], pcx[:, 0:N]), reads=[bcx], writes=[b_cx])
                pcc = bank(2 + fmc[0] % 2); bcc = b_ps[2 + fmc[0] % 2]; fmc[0] += 1
                p.op("pe", fmm(2560 + j * 128, pcc), reads=rHT + [b_winf], writes=[bcc])
                p.op("dve", I_tt(uT_st[:, j, 0:N], pcc[:, 0:N], cx_sb[:, 0:N], ALU.mult),
                     reads=[bcc, b_cx], writes=[b_uT] if j == 0 else [])
                pcb = bank(2 + fmc[0] % 2); bcb = b_ps[2 + fmc[0] % 2]; fmc[0] += 1
                p.op("pe", fmm(2048 + j * 128, pcb), reads=rHT + [b_winf], writes=[bcb])
                p.op("act", I_acopy(cbT_st[:, j, 0:N], pcb[:, 0:N]), reads=[bcb], writes=[b_cbT] if j == 0 else [])
                if j == 3:
                    b_uT.w = [("dve", p.cnt["dve"])]
                    b_cbT.w = [("act", p.cnt["act"])]
                    NV = N if T < 16 else NMETA
                    ucol = (17 + n0) if T < 16 else 1
                    p.dma("pool", I_dma(uT_d[:, :, ucol:ucol + NV].rearrange("j p n -> p j n"), uT_st[:, :, 0:NV]),
                          st_u, reads=[b_uT])
                    p.dma("pool", I_dma(cbT_d[:, :, n0:n0 + N].rearrange("j p n -> p j n"), cbT_st[:, :, 0:N]),
                          st_cb, reads=[b_cbT])

            for j in range(4):
                out.append(lambda j=j: fm_chunk(j))

            tmc = [0]
            deferred = []
            state = {"first_qk": True, "first_v": True}

            def qk_transposes(s, grp):
                pq = bank(6 + (s * 2 + grp) % 2).bitcast(BF16)[:, 0:512].rearrange("p (h t) -> p h t", t=128)
                bpq = b_ps[6 + (s * 2 + grp) % 2]
                p.op("pe", I_trs([(pq[:, h, :], qk_tm[:, s, grp, h * 128:(h + 1) * 128], ident_b)
                                  for h in range(4)]),
                     reads=[b_qktm[s][grp], b_ident], writes=[bpq])
                p.op("act", I_acopy(qkT_st[:, grp * 4:(grp + 1) * 4, s * 128:(s + 1) * 128], pq),
                     reads=[bpq], writes=[b_qkT] if state["first_qk"] else [])
                state["first_qk"] = False

            def tm_chunk(s, grp):
                pt = bank(4 + tmc[0] % 2); bpt = b_ps[4 + tmc[0] % 2]; tmc[0] += 1
                p.op("pe", I_mm([(pt, HT[:, c, s * 128:(s + 1) * 128], win_sb[:, c, grp * 512:(grp + 1) * 512],
                                  c == 0, c == 7, {}) for c in range(8)]),
                     reads=[bHT[s], b_win], writes=[bpt])
                if grp == 2:
                    p.op("act", I_acopy(v_st[:, s, :], pt), reads=[bpt], writes=[b_vst] if state["first_v"] else [])
                    state["first_v"] = False
                else:
                    rt = rtqT if grp == 0 else rtkT
                    psq = pt.rearrange("p (g d) -> p g d", d=64)
                    c2b = rt[:, s, 0:64].unsqueeze(1).to_broadcast([128, 8, 64])
                    nsb = rt[:, s, 64:96].unsqueeze(1).to_broadcast([128, 8, 32])
                    psb = rt[:, s, 96:128].unsqueeze(1).to_broadcast([128, 8, 32])
                    p.op("dve", I_tt(rt1, psq, c2b, ALU.mult), reads=[bpt, bRT], writes=[b_r1])
                    p.op("dve", I_tt(rt2[:, :, 0:32], psq[:, :, 32:64], nsb, ALU.mult), reads=[bpt, bRT],
                         writes=[b_r2])
                    tk2 = p.op("dve", I_tt(rt2[:, :, 32:64], psq[:, :, 0:32], psb, ALU.mult), reads=[bpt, bRT])
                    b_r2.w.append(tk2)
                    dst = qk_tm[:, s, grp, :].rearrange("p (g d) -> p g d", d=64)
                    p.op("dve", I_tt(dst, rt1, rt2, ALU.add), reads=[b_r1, b_r2], writes=[b_qktm[s][grp]])
                    deferred.append((s, grp))
                while len(deferred) > 2:
                    qk_transposes(*deferred.pop(0))

            for s in range(nsub):
                for grp in range(3):
                    out.append(lambda s=s, grp=grp: tm_chunk(s, grp))

            def finish():
                while deferred:
                    qk_transposes(*deferred.pop(0))
                b_qkT.w = [("act", p.cnt["act"])]
                b_vst.w = [("act", p.cnt["act"])]
                p.dma("pool", I_dma(qT_d[:, :, n0:n0 + N].rearrange("h p n -> p h n"), qkT_st[:, 0:4, 0:N]), st_qk,
                      reads=[b_qkT])
                p.dma("pool", I_dma(kT_d[:, :, n0:n0 + N].rearrange("h p n -> p h n"), qkT_st[:, 4:8, 0:N]), st_qk,
                      reads=[b_qkT])
                for h in range(4):
                    p.dma("pool", I_dma(v_d[h, :, T * 4:T * 4 + nsub, :], v_st[:, 0:nsub, h * 128:(h + 1) * 128]),
                          st_v, reads=[b_vst])
            out.append(finish)
            return out

        load_x(0, None, xt, xsem, b_xt)
        stageA(0)
        for T in range(NSUP):
            if T + 1 < NSUP:
                load_x(T + 1, None, xt, xsem, b_xt)
            ch = chunksB(T)
            half = 5
            for f_ in ch[:half]:
                f_()
            if T + 1 < NSUP:
                stageA(T + 1)
            for f_ in ch[half:]:
                f_()
        p.barrier()
        ar.top = persist_top

        QT = [ar.alloc([LP], BF16) for _ in range(2)]
        KT = [ar.alloc([LP], BF16) for _ in range(2)]
        VA = [ar.alloc([NT, 129], BF16) for _ in range(2)]
        Eb = [ar.alloc([2, 512], BF16) for _ in range(3)]
        gsub_b = ar.alloc([128])
        tmp_t = ar.alloc([128])
        o_s = ar.alloc([4, 128])
        on_s = ar.alloc([4, 128], BF16)
        jk = ar.alloc([128])
        c2 = ar.alloc([32])
        accs = ar.alloc([3, 408])
        aT_st = [ar.alloc([512], BF16) for _ in range(2)]
        b_qkv = [Buf(), Buf()]
        b_E = [Buf() for _ in range(3)]
        b_S = [Buf(), Buf()]
        b_acc = Buf()
        b_gs = Buf()
        b_c2 = Buf(); b_tt = Buf(); b_o = Buf(); b_on = Buf(); b_jk = Buf(); b_ms = Buf(); b_ms2 = Buf(); b_rs = Buf()
        b_accs = Buf()
        b_p7 = Buf()
        b_aT = [Buf(), Buf()]
        hsem = [p.new_dsem(), p.new_dsem()]
        st2 = [p.new_dsem(), p.new_dsem()]

        p.load1(gsub_b, gsub_d, b_gs)
        p.op("dve", I_ts(gsub_b, gsub_b, float(1.0 - LAM_INIT), None, ALU.mult), reads=[], writes=[b_gs])

        convw = ar.alloc([4, 3])
        cut = [ar.alloc([4, 514]) for _ in range(2)]
        ccb = [ar.alloc([4, 512]) for _ in range(2)]
        cca = ar.alloc([512])
        ccb2 = ar.alloc([512])
        cv_st = [ar.alloc([4, 512], BF16) for _ in range(2)]
        b_cw = Buf(); b_cut = [Buf(), Buf()]; b_ccb = [Buf(), Buf()]; b_cca = Buf(); b_ccb2 = Buf()
        b_cv = [Buf(), Buf()]
        ld_cut = [p.new_dsem(), p.new_dsem()]
        ld_ccb = [p.new_dsem(), p.new_dsem()]
        st_cv = [p.new_dsem(), p.new_dsem()]
        p.load1(convw, convw_d, b_cw)
        p.op("dve", I_memset(cv_st[0], 0.0), writes=[b_cv[0]])
        p.op("dve", I_memset(cv_st[1], 0.0), writes=[b_cv[1]])

        def conv_load(T):
            nsub = 4 if T < 16 else 1
            N = nsub * 128
            n0 = T * 512
            NV = N if T < 16 else NMETA
            sl = T % 2
            ucol = (16 + n0) if T < 16 else 0
            p.dma("sp", I_dma(cut[sl][:, :, 0:NV + 2], uT_d[:, :, ucol:ucol + NV + 2].rearrange("j p n -> p j n")),
                  ld_cut[sl], writes=[b_cut[sl]])
            p.dma("sp", I_dma(ccb[sl][:, :, 0:N], cbT_d[:, :, n0:n0 + N].rearrange("j p n -> p j n")), ld_ccb[sl],
                  writes=[b_ccb[sl]])

        def conv_compute(T):
            nsub = 4 if T < 16 else 1
            N = nsub * 128
            n0 = T * 512
            NV = N if T < 16 else NMETA
            sl = T % 2
            U = cut[sl]
            for j in range(4):
                p.op("dve", I_ts(cca[:, 0:NV], U[:, j, 0:NV], convw[:, j, 0:1], None, ALU.mult),
                     reads=[b_cut[sl], b_cw], writes=[b_cca])
                p.op("dve", I_stt(ccb2[:, 0:NV], U[:, j, 1:NV + 1], convw[:, j, 1:2], cca[:, 0:NV], ALU.mult, ALU.add),
                     reads=[b_cut[sl], b_cca], writes=[b_ccb2])
                p.op("dve", I_stt(cca[:, 0:NV], U[:, j, 2:NV + 2], convw[:, j, 2:3], ccb2[:, 0:NV], ALU.mult, ALU.add),
                     reads=[b_cut[sl], b_ccb2], writes=[b_cca])
                p.op("dve", I_tt(cv_st[sl][:, j, 0:NV], cca[:, 0:NV], ccb[sl][:, j, 0:NV], ALU.mult),
                     reads=[b_cca, b_ccb[sl]], writes=[b_cv[sl]] if j == 0 else [])
            b_cv[sl].w = [("dve", p.cnt["dve"])]
            p.dma("pool", I_dma(convT_d[:, :, n0:n0 + N].rearrange("j p n -> p j n"), cv_st[sl][:, :, 0:N]), st_cv[sl],
                  reads=[b_cv[sl]])
        b_va1 = [Buf(), Buf()]
        for sl in range(2):
            p.op("pool", I_memset(VA[sl][:, :, 128:129], 1.0), writes=[b_va1[sl]])
            p.op("pool", I_memset(VA[sl][:, NT - 1, 128:129], 0.0), writes=[b_va1[sl]])
            p.op("pool", I_memset(VA[sl][0:16, NT - 1, 128:129], 1.0), writes=[b_va1[sl]])

        def load_head(h):
            sl = h % 2
            p.dma("sp", I_dma(QT[sl], qT_d[h]), hsem[sl], writes=[b_qkv[sl]])
            t1_ = p.dma("sp", I_dma(KT[sl], kT_d[h]), hsem[sl])
            t2_ = p.dma("sp", I_dma(VA[sl][:, :, 0:128], v_d[h]), hsem[sl])
            b_qkv[sl].w += [t1_, t2_]

        def S2(sb):
            return bank(2 * sb, 2).rearrange("p (a c) -> p a c", c=512)

        def acc_ap(i, s, nsub=4):
            a = i * nsub + s
            bk = bank(4 + a // 3)
            o = (a % 3) * 129
            return bk[:, o:o + 129]

        items = [(h, qb, kt) for h in range(4) for qb in range(17) for kt in range(NT)]

        Qp = [ar.alloc([2, 512], BF16) for _ in range(2)]
        b_qp = [Buf(), Buf()]
        for sl_ in range(2):
            p.op("dve", I_memset(Qp[sl_], 0.0), writes=[b_qp[sl_]])

        def emit_qpad(h, qb):
            sl = h % 2
            qs = (h * 17 + qb) % 2
            q0 = qb * 512
            nq = 512 if qb < 16 else 128
            p.op("dve", I_copy(Qp[qs][0:64, 0, 0:nq], QT[sl][0:64, q0:q0 + nq]), reads=[b_qkv[sl]],
                 writes=[b_qp[qs]])
            tk_ = p.op("dve", I_copy(Qp[qs][64:128, 1, 0:nq], QT[sl][64:128, q0:q0 + nq]), reads=[b_qkv[sl]])
            b_qp[qs].w.append(tk_)

        def emit_qk(idx):
            h, qb, kt = items[idx]
            sl = h % 2
            sb = idx % 2
            qs = (h * 17 + qb) % 2
            nq = 512 if qb < 16 else 128
            s2 = S2(sb)
            p.op("pe", I_mm([
                (s2[:, 0, 0:nq], KT[sl][:, kt * 128:(kt + 1) * 128], Qp[qs][:, 0, 0:nq], True, True, {}),
                (s2[:, 1, 0:nq], KT[sl][:, kt * 128:(kt + 1) * 128], Qp[qs][:, 1, 0:nq], True, True, {})]),
                 reads=[b_qkv[sl], b_qp[qs]], writes=[b_S[sb]])

        def emit_exp_av(idx, nxt=None):
            h, qb, kt = items[idx]
            sl = h % 2
            sb = idx % 2
            eb = idx % 3
            nq = 512 if qb < 16 else 128
            nsub = nq // 128
            na = nq if qb < 16 else NMETA
            p.op("act", I_act(Eb[eb][:, :, 0:na], S2(sb)[:, :, 0:na], AF.Exp), reads=[b_S[sb]], writes=[b_E[eb]])
            if nxt is not None:
                emit_qk(nxt)
            lst = []
            seen = set()
            for i in range(2):
                for s in range(nsub):
                    bk_ = (i * nsub + s) // 3
                    st_ = (kt == 0) and (bk_ not in seen)
                    seen.add(bk_)
                    lst.append((acc_ap(i, s, nsub), Eb[eb][:, i, s * 128:(s + 1) * 128], VA[sl][:, kt, :],
                                st_, kt == NT - 1, dict(skip_group_check=True)))
            extra = list(b_acc.r) if kt == 0 else []
            tok = p.op("pe", I_mm(lst), reads=[b_E[eb], b_qkv[sl], b_va1[sl]], extra=extra)
            if kt == NT - 1:
                b_acc.w = [tok]
                b_acc.r = []

        epi_cnt = [0]
        pending = []

        def acc_sb(i, s_, nsub=4):
            a_ = i * nsub + s_
            o_ = (a_ % 3) * 129
            return accs[:, a_ // 3, o_:o_ + 129]

        def emit_epilogue(h, qb, idx):
            q0 = qb * 512
            nq = 512 if qb < 16 else 128
            nsub = nq // 128
            sts = epi_cnt[0] % 2
            epi_cnt[0] += 1
            nacc = 2 * nsub
            toksA = []
            for bk in range((nacc + 2) // 3):
                w_ = 129 * min(3, nacc - 3 * bk)
                toksA.append(p.op("dve", I_copy(accs[:, bk, 0:w_], bank(4 + bk)[:, 0:w_]), reads=[b_acc],
                                  writes=[b_accs] if bk == 0 else []))
            b_accs.w = toksA
            for s_ in range(nsub):
                a0 = acc_sb(0, s_, nsub)
                a1 = acc_sb(1, s_, nsub)
                p.op("dve", I_recip(c2[:, 0:1], a0[:, 128:129]), reads=[b_accs], writes=[b_c2])
                p.op("dve", I_recip(c2[:, 1:2], a1[:, 128:129]), reads=[b_accs], writes=[b_c2])
                p.op("dve", I_tt(c2[:, 2:3], c2[:, 1:2], lam_col, ALU.mult), reads=[b_lam], writes=[b_c2])
                p.op("dve", I_ts(tmp_t, a1[:, 0:128], c2[:, 2:3], None, ALU.mult), reads=[b_accs, b_c2],
                     writes=[b_tt])
                p.op("dve", I_stt(o_s[:, s_, :], a0[:, 0:128], c2[:, 0:1], tmp_t, ALU.mult, ALU.subtract),
                     reads=[b_accs, b_c2, b_tt], writes=[b_o])
                p.op("dve", I_stt(jk, o_s[:, s_, :], 1.0, o_s[:, s_, :], ALU.mult, ALU.mult,
                                  accum=c2[:, 8 + s_:9 + s_]), reads=[b_o], writes=[b_jk, b_ms])
            p.op("dve", I_ts(c2[:, 12:12 + nsub], c2[:, 8:8 + nsub], 1.0 / 128.0, EPS, ALU.mult, ALU.add),
                 reads=[b_ms], writes=[b_ms2])

            def stage_cd():
                p.op("act", I_act(c2[:, 12:12 + nsub], c2[:, 12:12 + nsub], AF.Ln), writes=[b_ms2])
                p.op("act", I_act(c2[:, 16:16 + nsub], c2[:, 12:12 + nsub], AF.Exp, scale=-0.5), reads=[b_ms2],
                     writes=[b_rs])
                for s_ in range(nsub):
                    p.op("dve", I_stt(on_s[:, s_, :], o_s[:, s_, :], c2[:, 16 + s_:17 + s_], gsub_b, ALU.mult,
                                      ALU.mult), reads=[b_o, b_rs, b_gs], writes=[b_on])

            def stage_e():
                p7 = bank(7).bitcast(BF16)[:, 0:512].rearrange("p (s t) -> p s t", t=128)
                p.op("pe", I_trs([(p7[:, s_, :], on_s[:, s_, :], ident_b) for s_ in range(nsub)]),
                     reads=[b_on, b_ident], writes=[b_p7])
                p.op("dve", I_copy(aT_st[sts][:, 0:nq], bank(7).bitcast(BF16)[:, 0:nq]), reads=[b_p7],
                     writes=[b_aT[sts]])
                p.dma("pool", I_dma(attnT_d[h, :, q0:q0 + nq], aT_st[sts][:, 0:nq]), st2[sts], reads=[b_aT[sts]])

            pending.append((idx + 14, stage_cd))
            pending.append((idx + 26, stage_e))

        load_head(0)
        load_head(1)
        conv_load(0)
        emit_scratch_init()
        emit_qpad(0, 0)
        emit_qk(0)
        emit_qk(1)
        for idx in range(len(items)):
            h, qb, kt = items[idx]
            if kt == 0 and idx + NT < len(items):
                nh, nqb, _ = items[idx + NT]
                emit_qpad(nh, nqb)
            emit_exp_av(idx, idx + 2 if idx + 2 < len(items) else None)
            if h == 0 and kt == 5 and qb + 1 < NSUP:
                conv_load(qb + 1)
            if h == 0 and kt == 30:
                conv_compute(qb)
            while pending and pending[0][0] <= idx:
                pending.pop(0)[1]()
            if kt == NT - 1:
                emit_epilogue(h, qb, idx)
                if qb == 16 and h + 2 < 4:
                    load_head(h + 2)
        while pending:
            pending.pop(0)[1]()
        p.barrier()
        ar.top = persist_top

        wout_sb = ar.alloc([8, 1024], BF16)
        wr_sb = ar.alloc([8, 16])
        wr_hi = ar.alloc([8, 16], BF16)
        wr_lo = ar.alloc([8, 16], BF16)
        g2b = ar.alloc([1024])
        affT = ar.alloc([LP])
        tokid = ar.alloc([NT])
        p34_top = ar.top
        mixT = [ar.alloc([8, 512], BF16) for _ in range(2)]
        xt3 = [ar.alloc([4, 1024]) for _ in range(2)]
        h1 = [ar.alloc([1024]) for _ in range(4)]
        hsf = [ar.alloc([1024]) for _ in range(2)]
        hhi = [ar.alloc([1024], BF16) for _ in range(2)]
        hlo = [ar.alloc([1024], BF16) for _ in range(2)]
        hlT = [ar.alloc([16, 128], BF16) for _ in range(2)]
        c3 = ar.alloc([64])
        sqjunk = ar.alloc([1024], BF16)
        b_sqj = Buf()
        ex = [ar.alloc([16]) for _ in range(2)]

        b_wout = Buf(); b_wr = Buf(); b_g2 = Buf(); b_cw = Buf(); b_tok = Buf(); b_comb = Buf(); b_affT = Buf()
        b_mix = [Buf(), Buf()]
        b_mixc = [[Buf() for _ in range(4)] for _ in range(2)]
        b_ut = [Buf(), Buf()]; b_cbt = [Buf(), Buf()]; b_ca = Buf(); b_cb2 = Buf()
        b_xt3 = [Buf(), Buf()]
        b_h1 = [Buf() for _ in range(4)]
        b_hsf = [Buf(), Buf()]; b_hhi = [Buf(), Buf()]; b_hlo = [Buf(), Buf()]; b_hlT = [Buf(), Buf()]
        b_ss3 = [Buf(), Buf()]; b_t3 = [Buf(), Buf()]; b_r3 = [Buf(), Buf()]
        b_lg = [Buf(), Buf()]; b_ex = [Buf(), Buf()]; b_se = [Buf(), Buf()]
        b_po = [Buf(), Buf()]; b_pT = Buf(); b_plg = [Buf(), Buf()]; b_paT = [Buf(), Buf()]
        wsem3 = p.new_dsem()
        ld3 = [p.new_dsem(), p.new_dsem()]
        ld_ut = [p.new_dsem(), p.new_dsem()]
        ld_cbt = [p.new_dsem(), p.new_dsem()]
        st_h1 = [p.new_dsem() for _ in range(4)]
        st_hs = [p.new_dsem(), p.new_dsem()]
        xsem3 = [p.new_dsem(), p.new_dsem()]

        woutv = wout_d.rearrange("(c p) n -> p c n", p=128)
        for c in range(8):
            p.dma("pool", I_dma(wout_sb[:, c, :], woutv[:, c, :]), wsem3)
        b_wout.w = [(wsem3, wsem3.total)]
        p.load1(wr_sb, wr_d.rearrange("(c p) e -> p c e", p=128), b_wr)
        b_wrh = Buf()
        p.op("dve", I_copy(wr_hi, wr_sb), reads=[b_wr], writes=[b_wrh])
        p.op("dve", I_tt(wr_lo, wr_sb, wr_hi, ALU.subtract), reads=[b_wr, b_wrh], writes=[])
        b_wr.w = [("dve", p.cnt["dve"])]
        p.load1(g2b, g2_d, b_g2)
        p.load1(tokid, tokid_d, b_tok)
        p.op("dve", I_copy(comb[:, :, :, 0], tokid.unsqueeze(2).to_broadcast([128, NT, NE])), reads=[b_tok],
             writes=[b_comb])
        p.op("pool", I_memset(mixT[0], 0.0), writes=[b_mix[0]])
        p.op("pool", I_memset(mixT[1], 0.0), writes=[b_mix[1]])

        def sup(T):
            nsub = 4 if T < 16 else 1
            return nsub, nsub * 128, T * 512, (nsub * 128 if T < 16 else NMETA)

        def load3(T):
            nsub, N, n0, NV = sup(T)
            sl = T % 2
            p.dma("sp", I_dma(mixT[sl][:, 0:4, 0:N], attnT_d[:, :, n0:n0 + N].rearrange("h p n -> p h n")), ld3[sl],
                  writes=[b_mix[sl]])
            load_x(T, None, xt3, xsem3, b_xt3)
            p.dma("sp", I_dma(mixT[sl][:, 4:8, 0:N], convT_d[:, :, n0:n0 + N].rearrange("j p n -> p j n")), ld3[sl])
            b_mix[sl].w.append((ld3[sl], ld3[sl].total))

        subs = []
        for T in range(NSUP):
            for s in range(4 if T < 16 else 1):
                subs.append((T, s))
        NJ = len(subs)

        def S0(j):
            T, s = subs[j]
            sl = T % 2
            MT = mixT[sl]
            po = bank(2 * (j % 2), 2)
            lst = []
            for half in range(2):
                for m in range(8):
                    lst.append((po[:, half * 512:(half + 1) * 512], MT[:, m, s * 128:(s + 1) * 128],
                                wout_sb[:, m, half * 512:(half + 1) * 512], m == 0, m == 7, {}))
            p.op("pe", I_mm(lst), reads=[b_mix[sl], b_wout] + b_mixc[sl], writes=[b_po[j % 2]])

        def S1(j):
            T, s = subs[j]
            sl = T % 2
            hb = j % 2
            r0 = T * 512 + s * 128
            h4 = j % 4
            p.op("dve", I_tt(h1[h4], bank(2 * hb, 2), xt3[sl][:, s, :], ALU.add), reads=[b_po[hb], b_xt3[sl]],
                 writes=[b_h1[h4]])
            p.dma("pool", I_dma(hacc_d[r0:r0 + 128, :], h1[h4]), st_h1[h4], reads=[b_h1[h4]])
            if h1dbg_d is not None:
                p.dma("pool", I_dma(h1dbg_d[r0:r0 + 128, :], h1[h4]), st_h1[h4], reads=[b_h1[h4]])
            p.op("act", I_act(sqjunk, h1[h4], AF.Square, accum_out=c3[:, hb:hb + 1]), reads=[b_h1[h4]],
                 writes=[b_sqj, b_ss3[hb]])

        def S2(j):
            hb = j % 2
            rstd_chain(c3[:, hb:hb + 1], float(D), c3[:, 2 + hb:3 + hb], c3[:, 4 + hb:5 + hb], b_ss3[hb], b_t3[hb],
                       b_r3[hb])

        def S3a(j):
            T, s = subs[j]
            hb = j % 2
            r0 = T * 512 + s * 128
            p.op("dve", I_stt(hsf[hb], h1[j % 4], c3[:, 4 + hb:5 + hb], g2b, ALU.mult, ALU.mult),
                 reads=[b_h1[j % 4], b_r3[hb], b_g2], writes=[b_hsf[hb]])
            p.op("act", I_acopy(hhi[hb], hsf[hb]), reads=[b_hsf[hb]], writes=[b_hhi[hb]])
            p.dma("pool", I_dma(hs_d[r0:r0 + 128, :], hhi[hb]), st_hs[hb], reads=[b_hhi[hb]])

        def S3b(j):
            hb = j % 2
            p.op("dve", I_tt(hlo[hb], hsf[hb], hhi[hb], ALU.subtract), reads=[b_hsf[hb], b_hhi[hb]],
                 writes=[b_hlo[hb]])

        def S4(j):
            hb = j % 2
            pT = bank(4, 2).bitcast(BF16).rearrange("p (c t) -> p c t", t=128)
            lst = [(pT[:, c, :], hhi[hb][:, c * 128:(c + 1) * 128], ident_b) for c in range(8)]
            lst += [(pT[:, 8 + c, :], hlo[hb][:, c * 128:(c + 1) * 128], ident_b) for c in range(8)]
            p.op("pe", I_trs(lst), reads=[b_hhi[hb], b_hlo[hb], b_ident], writes=[b_pT])
            p.op("act", I_acopy(hlT[hb], pT), reads=[b_pT], writes=[b_hlT[hb]])

        def S5a(j):
            hb = j % 2
            plg = bank(6)[:, hb * 16:(hb + 1) * 16]
            lst = []
            for c in range(8):
                lst.append((plg, hlT[hb][:, c, :], wr_hi[:, c, :], c == 0, False, {}))
                lst.append((plg, hlT[hb][:, c, :], wr_lo[:, c, :], False, False, {}))
                lst.append((plg, hlT[hb][:, 8 + c, :], wr_hi[:, c, :], False, c == 7, {}))
            p.op("pe", I_mm(lst), reads=[b_hlT[hb], b_wr], writes=[b_plg[hb]])
            p.op("dve", lambda e, plg=plg, hb=hb: e.reduce_max(out=c3[:, 8 + hb:9 + hb], in_=plg,
                                                               axis=mybir.AxisListType.X),
                 reads=[b_plg[hb]], writes=[b_lg[hb]])
            p.op("dve", I_ts(c3[:, 10 + hb:11 + hb], c3[:, 8 + hb:9 + hb], -1.0, None, ALU.mult), reads=[b_lg[hb]],
                 writes=[b_lg[hb]])
            p.op("act", I_act(ex[hb], plg, AF.Exp, bias=c3[:, 10 + hb:11 + hb], accum_out=c3[:, 12 + hb:13 + hb]),
                 reads=[b_plg[hb], b_lg[hb]], writes=[b_ex[hb], b_se[hb]])

        def S5b(j):
            hb = j % 2
            p.op("dve", I_recip(c3[:, 14 + hb:15 + hb], c3[:, 12 + hb:13 + hb]), reads=[b_se[hb]], writes=[b_se[hb]])
            p.op("dve", I_ts(comb[:, j, :, 1], ex[hb], c3[:, 14 + hb:15 + hb], None, ALU.mult),
                 reads=[b_ex[hb], b_se[hb]], writes=[b_comb])

        def S6(j):
            hb = j % 2
            paT = bank(7)[0:16, hb * 128:(hb + 1) * 128]
            p.op("pe", I_trs([(paT, comb[:, j, :, 1], ident_f)]), reads=[b_comb, b_ident], writes=[b_paT[hb]])
            p.op("dve", I_copy(affT[0:16, j * 128:(j + 1) * 128], paT), reads=[b_paT[hb]], writes=[])

        stages = [S0, S1, S2, S3a, S3b, S4, S5a, S5b, S6]
        load3(0)
        for t in range(NJ + len(stages) - 1):
            for k in reversed(range(len(stages))):
                j = t - k
                if 0 <= j < NJ:
                    stages[k](j)
            if t < NJ:
                T, s = subs[t]
                if s == 0 and T + 1 < NSUP:
                    load3(T + 1)
        b_affT.w = [("dve", p.cnt["dve"])]
        p.barrier()
        ar.top = p34_top

        ar.top = p34_top
        wslot = [ar.alloc_top([8, 1024], BF16) for _ in range(6)]
        b_ws = [Buf() for _ in range(6)]
        wsem5 = [p.new_dsem() for _ in range(6)]

        def load_w(e_, k):
            if k < 4:
                src = (wg_d if k % 2 == 0 else wu_d)[e_].rearrange("(c p) f -> p c f", p=128)
                fh = k // 2
                src = src[:, :, fh * 1024:(fh + 1) * 1024]
            else:
                fh = k - 4
                src = wd_d[e_].rearrange("(c p) d -> p c d", p=128)[:, fh * 8:(fh + 1) * 8, :]
            for c in range(0, 8, 2):
                p.dma("pool", I_dma(wslot[k][:, c:c + 2, :], src[:, c:c + 2, :]), wsem5[k],
                      writes=[b_ws[k]] if c == 0 else [])
            b_ws[k].w = [(wsem5[k], wsem5[k].total)]

        for k in range(6):
            load_w(0, k)

        SEG = L // 8
        A128 = ar.alloc([SEG])
        msk = ar.alloc([SEG])
        cum = ar.alloc([SEG])
        ones = ar.alloc([SEG])
        gmat = ar.alloc([128])
        tmat = ar.alloc([128])
        c4 = ar.alloc([16])
        slotf = affT
        b4 = Buf()
        b_A = Buf(); b_gm = Buf(); b_tm = Buf(); b_cnt = Buf(); b_tot = Buf(); b_sl = Buf(); b_si = Buf()
        b_p4 = Buf(); b_s128 = Buf()
        bsem = [p.new_dsem() for _ in range(4)]
        p.load1(gmat, gmat_d, b_gm)
        p.load1(tmat, tmat_d, b_tm)
        t_st = p.dma("sp", I_dma(aff_d, affT[0:16, 0:L]), bsem[0], reads=[b_affT])
        p.dma("sp", I_dma(A128, aff_d.rearrange("e (s c) -> (e s) c", c=SEG)), bsem[1], writes=[b_A], extra=[t_st])
        lo = c4[:, 0:1]
        mid = c4[:, 1:2]
        cnt = c4[:, 2:3]
        dl = c4[:, 3:4]
        tot_ps = bank(3)[:, 0:1]
        p.op("dve", I_memset(c4, 0.0), writes=[b4])
        p.op("dve", I_memset(ones, 1.0), writes=[b4])
        junk3 = ar.alloc([SEG])
        w3 = c4[:, 5:8]
        tvec = c4[:, 8:11]
        cnt3 = c4[:, 11:14]
        ssum = c4[:, 14:15]
        ge3 = ar.alloc([8])
        tot3_ps = bank(3)[:, 0:3]
        b_tv = Buf(); b_c3 = Buf(); b_s = Buf(); b_lo = Buf()
        for i_ in range(3):
            p.op("dve", I_memset(c4[:, 5 + i_:6 + i_], float(i_ + 1)), writes=[b4])
        b_lo.w = list(b4.w)
        jbufs = [msk, cum, junk3]
        for k in range(NBIS // 2):
            w = 4.0 ** -(k + 1)
            p.op("dve", I_ts(tvec, w3, w, lo, ALU.mult, ALU.add), reads=[b_lo], writes=[b_tv])
            toks = []
            for i_ in range(3):
                toks.append(p.op("dve", I_ts(jbufs[i_], A128, tvec[:, i_:i_ + 1], 0.0, ALU.is_ge, ALU.add,
                                             accum=cnt3[:, i_:i_ + 1]),
                                 reads=[b_A, b_tv], writes=[b_c3] if i_ == 0 else []))
            b_c3.w = toks
            p.op("pe", I_mm([(tot3_ps, gmat, cnt3, True, True, {})]), reads=[b_c3, b_gm], writes=[b_tot])
            p.op("dve", I_ts(ge3[:, 0:3], tot3_ps, float(CAP) - 0.5, 0.0, ALU.is_ge, ALU.add, accum=ssum),
                 reads=[b_tot], writes=[b_s])
            p.op("dve", I_stt(lo, ssum, w, lo, ALU.mult, ALU.add), reads=[b_s], writes=[b_lo])
        b4.w = list(b_lo.w) + list(b_c3.w)
        p.op("dve", I_ts(msk, A128, lo, None, ALU.is_ge), reads=[b_A], writes=[b4])
        p.op("dve", lambda e: e.tensor_tensor_scan(out=cum, data0=ones, data1=msk, initial=0.0, op0=ALU.mult,
                                                   op1=ALU.add), writes=[b4, b_cnt])
        p.op("pe", I_mm([(tot_ps, tmat, cum[:, SEG - 1:SEG], True, True, {})]), reads=[b_cnt, b_tm], writes=[b_tot])
        p.op("dve", I_ts(c4[:, 4:5], tot_ps, -(BIG + 1.0), None, ALU.add), reads=[b_tot], writes=[b4])
        p.op("dve", I_stt(cum, cum, c4[:, 4:5], msk, ALU.add, ALU.mult), writes=[b4])
        p.op("dve", I_ts(cum, cum, BIG, None, ALU.add), writes=[b4, b_s128])
        t_st2 = p.dma("sp", I_dma(aff_d.rearrange("e (s c) -> (e s) c", c=SEG), cum), bsem[2], reads=[b_s128],
                      extra=[(bsem[1], bsem[1].total)])
        p.op("dve", I_memset(slotf[0:16, L:LP], BIG), writes=[b4])
        t_ld2 = p.dma("sp", I_dma(slotf[0:16, 0:L], aff_d), bsem[3], extra=[t_st2, (bsem[0], bsem[0].total)])
        b_sl.w = [t_ld2, ("dve", p.cnt["dve"])]
        for g in range(3):
            j0 = g * 32
            j1 = min(NT, j0 + 32)
            pg = bank(g)
            p.op("pe", I_trs([(pg[:, (j - j0) * 16:(j - j0 + 1) * 16], slotf[0:16, j * 128:(j + 1) * 128],
                               ident_f[0:16, 0:16]) for j in range(j0, j1)]),
                 reads=[b_sl, b_ident], writes=[b_p4])
            p.op("dve", I_copy(slot_i[:, j0:j1, :], pg[:, 0:(j1 - j0) * 16].rearrange("p (j e) -> p j e", e=16)),
                 reads=[b_p4], writes=[b_si])
        csem = [p.new_dsem() for _ in range(NE)]

        def emit_comb_scatter(e_, j0=0, j1=NT):
            for j in range(j0, j1):
                def sc(e, e_=e_, j=j):
                    return e.indirect_dma_start(
                        out=comb_d[e_], out_offset=bass.IndirectOffsetOnAxis(ap=slot_i[:, j, e_:e_ + 1], axis=0),
                        in_=comb[:, j, e_, :], in_offset=None, bounds_check=bc_reg(e, CAPP - 1), oob_is_err=False)
                p.dma("pool", sc, csem[e_], reads=[b_si, b_comb])

        emit_comb_scatter(0)
        p.barrier_on([("dve", p.cnt["dve"]), ("pe", p.cnt["pe"]), ("act", p.cnt["act"])])
        ar.top = persist_top

        xg = ar.alloc([NCT, 1024], BF16)
        _xgT = ar.alloc([8, CAPP], BF16)
        xgT = [_xgT, _xgT]
        actT = ar.alloc([16, CAPP], BF16)
        sil = [ar.alloc([512]) for _ in range(2)]
        NY = 4
        y_st = [ar.alloc([1024]) for _ in range(NY)]
        cg = [ar.alloc([NCT, 2]) for _ in range(2)]
        idx_i = [ar.alloc([NCT], I32) for _ in range(2)]
        b_xg = [Buf() for _ in range(NCT)]
        _bx = [Buf() for _ in range(NCT)]
        b_xgT = [_bx, _bx]
        b_act = [Buf() for _ in range(16)]
        b_sil = [Buf(), Buf()]
        b_y = [Buf() for _ in range(4)]
        b_cg = [Buf(), Buf()]
        b_idx = [Buf(), Buf()]
        b_p5 = [Buf() for _ in range(8)]
        cgsem = [p.new_dsem(), p.new_dsem()]
        gsem = [p.new_dsem() for _ in range(NCT)]
        scsem = [p.new_dsem() for _ in range(4)]

        def emit_gather(e_):
            sl = e_ % 2
            p.dma("sp", I_dma(cg[sl], comb_d[e_].rearrange("(ct p) t -> p ct t", p=128)), cgsem[sl],
                  writes=[b_cg[sl]], extra=[(csem[e_], csem[e_].total)])
            p.op("dve", I_copy(idx_i[sl], cg[sl][:, :, 0]), reads=[b_cg[sl]], writes=[b_idx[sl]])
            for ct in range(NCT):
                def ga(e, sl=sl, ct=ct):
                    return e.indirect_dma_start(
                        out=xg[:, ct, :], out_offset=None, in_=hs_d,
                        in_offset=bass.IndirectOffsetOnAxis(ap=idx_i[sl][:, ct:ct + 1], axis=0),
                        bounds_check=bc_reg(e, ROWS - 1), oob_is_err=True)
                p.dma("pool", ga, gsem[ct], reads=[b_idx[sl]], writes=[b_xg[ct]])

        p.op("dve", I_memset(actT, 0.0), writes=b_act)
        tr_cnt = [0]

        def emit_xg_transposes(e_):
            sl = e_ % 2
            for ct in range(NCT):
                bk = 6 + tr_cnt[0] % 2
                tr_cnt[0] += 1
                pb = bank(bk).bitcast(BF16).rearrange("p (c t) -> p c t", t=128)
                p.op("pe", I_trs([(pb[:, c, :], xg[:, ct, c * 128:(c + 1) * 128], ident_b) for c in range(8)]),
                     reads=[b_xg[ct], b_ident], writes=[b_p5[bk]])
                p.op("act" if ct % 2 == 0 else "dve",
                     (I_acopy if ct % 2 == 0 else I_copy)(xgT[sl][:, :, ct * 128:(ct + 1) * 128], pb),
                     reads=[b_p5[bk]], writes=[b_xgT[sl][ct]])

        CG = [(0, 342), (342, 342), (684, 342)]
        gu_cnt = [0]

        def emit_gateup(e_, fh):
            sl = e_ % 2
            XT = xgT[sl]
            for fc in range(8):
                f = fh * 8 + fc
                for gi, (c0, n) in enumerate(CG):
                    ab = gu_cnt[0] % 2
                    gu_cnt[0] += 1
                    pA = bank(2 * ab)
                    pB = bank(2 * ab + 1)
                    rx = [b_xgT[sl][ct] for ct in range(c0 // 128, (c0 + n + 127) // 128)]
                    p.op("pe", I_mm([(pA[:, 0:n], wslot[fh * 2][:, c, fc * 128:(fc + 1) * 128], XT[:, c, c0:c0 + n],
                                      c == 0, c == 7, {}) for c in range(8)]),
                         reads=rx + [b_ws[fh * 2]], writes=[b_p5[2 * ab]])
                    p.op("pe", I_mm([(pB[:, 0:n], wslot[fh * 2 + 1][:, c, fc * 128:(fc + 1) * 128],
                                      XT[:, c, c0:c0 + n], c == 0, c == 7, {}) for c in range(8)]),
                         reads=rx + [b_ws[fh * 2 + 1]], writes=[b_p5[2 * ab + 1]])
                    p.op("act", I_act(sil[ab][:, 0:n], pA[:, 0:n], AF.Silu), reads=[b_p5[2 * ab]], writes=[b_sil[ab]])
                    tok = p.op("dve", I_tt(actT[:, f, c0:c0 + n], sil[ab][:, 0:n], pB[:, 0:n], ALU.mult),
                               reads=[b_sil[ab], b_p5[2 * ab + 1]], extra=list(b_act[f].r) if gi == 0 else [])
                    if gi == 2:
                        b_act[f].w = [tok]
                        b_act[f].r = []

        y_cnt = [0]

        def emit_down(e_, ct):
            sl = e_ % 2
            ys = y_cnt[0] % NY
            y_cnt[0] += 1
            for half in range(2):
                pY = bank(4 + half)
                p.op("pe", I_mm([(pY, actT[:, f, ct * 128:(ct + 1) * 128],
                                  wslot[4 + f // 8][:, f % 8, half * 512:(half + 1) * 512], f == 0, f == 15, {})
                                 for f in range(16)]),
                     reads=b_act + [b_ws[4], b_ws[5]], writes=[b_p5[4 + half]])
                p.op("dve", I_ts(y_st[ys][:, half * 512:(half + 1) * 512], pY, cg[sl][:, ct, 1:2], None, ALU.mult),
                     reads=[b_p5[4 + half], b_cg[sl]], writes=[b_y[ys]] if half == 0 else [])
            b_y[ys].w = [("dve", p.cnt["dve"])]

            def sa(e, sl=sl, ct=ct, ys=ys):
                return e.indirect_dma_start(
                    out=hacc_d, out_offset=bass.IndirectOffsetOnAxis(ap=idx_i[sl][:, ct:ct + 1], axis=0),
                    in_=y_st[ys], in_offset=None, bounds_check=bc_reg(e, ROWS - 1), oob_is_err=True, compute_op=ALU.add)
            extra = list(prev_sc[0]) if ct == 0 else []
            p.dma("pool", sa, scsem[ys], reads=[b_y[ys], b_idx[sl]], extra=extra)
            if ct == NCT - 1:
                prev_sc[0] = [(sc_, sc_.total) for sc_ in scsem if sc_.total > 0]

        prev_sc = [[]]
        emit_gather(0)
        emit_comb_scatter(1)
        emit_xg_transposes(0)
        for e_ in range(NE):
            if e_ + 2 < NE and e_ > 0:
                emit_comb_scatter(e_ + 2, 0, 36)
            emit_gateup(e_, 0)
            if e_ + 1 < NE:
                load_w(e_ + 1, 0)
                load_w(e_ + 1, 1)
            if e_ + 2 < NE and e_ > 0:
                emit_comb_scatter(e_ + 2, 36, NT)
            if e_ + 1 < NE and e_ > 0:
                emit_gather(e_ + 1)
            emit_gateup(e_, 1)
            if e_ + 1 < NE:
                load_w(e_ + 1, 2)
                load_w(e_ + 1, 3)
                if e_ == 0:
                    emit_gather(e_ + 1)
            for ct in range(NCT):
                emit_down(e_, ct)
                if ct == 3 and e_ + 1 < NE:
                    emit_xg_transposes(e_ + 1)
            if e_ + 1 < NE:
                load_w(e_ + 1, 4)
                load_w(e_ + 1, 5)
            if e_ == 0:
                emit_comb_scatter(2)
        p.barrier()
        ar.top = persist_top

        ar.n = ARENA_WORDS
        g3b = ar.alloc([1024])
        hx = [ar.alloc([4, 1024]) for _ in range(3)]
        ox = [ar.alloc([4, 1024]) for _ in range(3)]
        jk6 = ar.alloc([1024], BF16)
        c6 = ar.alloc([16])
        b_g3 = Buf(); b_hx = [Buf() for _ in range(3)]; b_ox = [Buf() for _ in range(3)]; b_j6 = Buf(); b_s6 = Buf(); b_t6 = Buf(); b_r6 = Buf()
        l6 = [p.new_dsem() for _ in range(3)]
        s6 = [p.new_dsem() for _ in range(3)]
        p.load1(g3b, g3_d, b_g3)

        def load6(T):
            p.dma("sp", I_dma(hx[T % 3], hacc_d[T * 512:(T + 1) * 512, :].rearrange("(s p) d -> p s d", p=128)),
                  l6[T % 3], writes=[b_hx[T % 3]])

        load6(0)
        load6(1)
        for T in range(16):
            if T + 2 < 16:
                load6(T + 2)
            X = hx[T % 3]
            for s in range(4):
                p.op("act", I_act(jk6, X[:, s, :], AF.Square, accum_out=c6[:, s:s + 1]), reads=[b_hx[T % 3]],
                     writes=[b_j6, b_s6] if s == 0 else [b_j6])
            b_s6.w = [("act", p.cnt["act"])]
            rstd_chain(c6[:, 0:4], float(D), c6[:, 4:8], c6[:, 8:12], b_s6, b_t6, b_r6)
            for s in range(4):
                p.op("dve", I_stt(ox[T % 3][:, s, :], X[:, s, :], c6[:, 8 + s:9 + s], g3b, ALU.mult, ALU.mult),
                     reads=[b_hx[T % 3], b_r6, b_g3], writes=[b_ox[T % 3]] if s == 0 else [])
            b_ox[T % 3].w = [("dve", p.cnt["dve"])]
            p.dma("pool", I_dma(out_d[T * 512:(T + 1) * 512, :].rearrange("(s p) d -> p s d", p=128), ox[T % 3]),
                  s6[T % 3], reads=[b_ox[T % 3]])
        p.barrier()

        with nc.Block() as block:
            @block.sync
            def _(e):
                p.replay("sp", e)

            @block.gpsimd
            def _(e):
                p.replay("pool", e)

            @block.scalar
            def _(e):
                p.replay("act", e)

            @block.vector
            def _(e):
                p.replay("dve", e)

            @block.tensor
            def _(e):
                p.replay("pe", e)
    return nc


_NC_CACHE = {}


def _rope_table(scale):
    n = np.arange(LP)
    pos = np.where(n < SEQ, n + NMETA, n - SEQ)
    pos = np.where(n < L, pos, 0).astype(np.float32)
    inv_freq = (np.float32(10000.0) ** (-(np.arange(0, 64, 2, dtype=np.float32)) / np.float32(64))).astype(np.float32)
    ang = (pos[:, None] * inv_freq[None, :]).astype(np.float32)
    cos = np.cos(ang).astype(np.float32)
    sin = np.sin(ang).astype(np.float32)
    t = np.concatenate([cos, cos, -sin, sin], axis=1).astype(np.float32) * np.float32(scale)
    return np.ascontiguousarray(t)


def kernel(x, meta_tokens, mix_norm_g, w_in, conv_w, lambda_q1, lambda_k1, lambda_q2, lambda_k2, attn_subln_g,
           w_out, ffn_norm_g, w_router, w_gate, w_up, w_down, final_norm_g):
    f = lambda a: np.ascontiguousarray(np.asarray(a, dtype=np.float32))
    x = f(x)
    B = x.shape[0]
    if "nc" not in _NC_CACHE:
        _NC_CACHE["nc"] = build_nc()
    nc = _NC_CACHE["nc"]
    tile128 = lambda v: np.ascontiguousarray(np.broadcast_to(f(v).reshape(1, -1), (128, f(v).size)))
    convw = np.ascontiguousarray(f(conv_w)[0].reshape(3, 4, 128).transpose(2, 1, 0))
    lamp = np.ascontiguousarray(np.broadcast_to(
        np.stack([f(lambda_q1)[0], f(lambda_k1)[0], f(lambda_q2)[0], f(lambda_k2)[0]])[None], (128, 4, 64)))
    tokid = np.ascontiguousarray((np.arange(NT)[None, :] * 128 + np.arange(128)[:, None]).astype(np.float32))
    pp = np.arange(128)
    same = (pp[:, None] // 8) == (pp[None, :] // 8)
    gmat = np.ascontiguousarray(same.astype(np.float32))
    tmat = np.ascontiguousarray((same & (pp[:, None] < pp[None, :])).astype(np.float32))
    dummy = np.zeros((128, NCT, 2), np.float32)
    dummy[:, :, 0] = (L + np.arange(128))[:, None]
    shared = {
        "meta": f(meta_tokens),
        "g1b": tile128(f(mix_norm_g)[0]), "g2b": tile128(f(ffn_norm_g)[0]), "g3b": tile128(f(final_norm_g)),
        "w_in": f(w_in)[0], "w_out": f(w_out)[0], "w_router": f(w_router)[0],
        "w_gate": f(w_gate)[0], "w_up": f(w_up)[0], "w_down": f(w_down)[0],
        "convw": convw, "lamp": lamp, "gsub": tile128(f(attn_subln_g)[0]),
        "ropeq": _rope_table(0.125), "ropek": _rope_table(1.0),
        "ident": np.eye(128, dtype=np.float32), "tokid": tokid, "dummyrows": dummy,
        "gmat": gmat, "tmat": tmat,
    }
    in_maps = []
    for b in range(B):
        m = dict(shared)
        m["x"] = x[b]
        in_maps.append(m)
    res = run_bass_kernel_spmd(nc, in_maps, core_ids=list(range(B)))
    if DEBUG:
        _NC_CACHE["dbg"] = {k: np.asarray(res.results[0][k]) for k in DEBUG}
    return np.stack([np.asarray(r["out"], dtype=np.float32) for r in res.results], axis=0)
```

```python
import contextlib
import math
import numpy as np
import concourse.bass as bass
import concourse.mybir as mybir
from concourse.bass_utils import run_bass_kernel_spmd

F32 = mybir.dt.float32
BF16 = mybir.dt.bfloat16
I32 = mybir.dt.int32
ALU = mybir.AluOpType
AF = mybir.ActivationFunctionType

D = 1024
SEQ = 8192
NMETA = 16
L = SEQ + NMETA
LP = 8320
NT = 65
NE = 16
CAP = 1026
CAPP = 1152
NCT = 9
DFF = 2048
ROWS = L + 128
EPS = 1e-6
LAM_INIT = 0.8 - 0.6 * math.exp(-0.3 * 0)
BIG = 5000.0
NBIS = 28
ARENA_WORDS = 53100


def _size(dt):
    return 2 if dt == BF16 else 4


class DSem:
    def __init__(self, h):
        self.h = h
        self.total = 0


class Buf:
    __slots__ = ("w", "r")

    def __init__(self):
        self.w = []
        self.r = []


class Prog:
    ENG = ("pe", "act", "dve", "pool", "sp")

    def __init__(self, nc, es):
        self.nc = nc
        self.es = es
        self.q = {k: [] for k in self.ENG}
        self.cnt = {k: 0 for k in ("pe", "act", "dve", "pool")}
        self.sem = {k: es.enter_context(nc.semaphore("s_" + k)) for k in self.cnt}
        self.dsems = []

    def new_dsem(self):
        h = self.es.enter_context(self.nc.semaphore("d%d" % len(self.dsems)))
        d = DSem(h)
        self.dsems.append(d)
        return d

    @staticmethod
    def _deps(reads, writes, extra):
        deps = list(extra)
        for b in reads:
            deps += b.w
        for b in writes:
            deps += b.w
            deps += b.r
        return deps

    @staticmethod
    def _commit(tok, reads, writes):
        for b in writes:
            b.w = [tok]
            b.r = []
        for b in reads:
            b.r = [t for t in b.r if t[0] != tok[0]] + [tok]

    def op(self, eng, fn, reads=(), writes=(), extra=()):
        deps = self._deps(reads, writes, extra)
        self.cnt[eng] += 1
        tok = (eng, self.cnt[eng])
        self.q[eng].append((fn, deps, tok, 1))
        self._commit(tok, reads, writes)
        return tok

    def dma(self, queue, fn, dsem, reads=(), writes=(), extra=()):
        deps = self._deps(reads, writes, extra)
        dsem.total += 16
        tok = (dsem, dsem.total)
        self.q[queue].append((fn, deps, tok, 16))
        self._commit(tok, reads, writes)
        return tok

    def barrier(self):
        toks = [(k, self.cnt[k]) for k in self.cnt if self.cnt[k] > 0]
        toks += [(d, d.total) for d in self.dsems if d.total > 0]
        for k in self.ENG:
            self.q[k].append((None, toks, None, 0))

    def barrier_on(self, toks):
        for k in self.ENG:
            self.q[k].append((None, list(toks), None, 0))

    def load1(self, out, in_, b):
        return self.dma("sp", I_dma(out, in_), self.new_dsem(), writes=[b])

    def _h(self, k):
        return self.sem[k] if isinstance(k, str) else k.h

    def replay(self, name, e):
        waited = {}
        for fn, deps, tok, inc in self.q[name]:
            need = {}
            for k, v in deps:
                if name == "pe" and k == "pe" and fn is not None:
                    continue
                if v > need.get(k, 0):
                    need[k] = v
            for k, v in need.items():
                if waited.get(k, 0) < v:
                    e.wait_ge(self._h(k), v)
                    waited[k] = v
            if fn is None:
                continue
            ins = fn(e)
            ins.then_inc(self._h(tok[0]), inc)


class Arena:
    def __init__(self, t, nwords):
        self.t = t
        self.n = nwords
        self.top = 0

    def alloc_top(self, shape, dt=F32):
        nel = int(np.prod(shape))
        nw = ((nel * _size(dt) + 3) // 4 + 7) // 8 * 8
        self.n -= nw
        save = self.top
        self.top = self.n
        self.n += nw
        a = self.alloc(shape, dt)
        self.n -= nw
        self.top = save
        assert self.top <= self.n
        return a

    def alloc(self, shape, dt=F32):
        nel = int(np.prod(shape))
        nw = (nel * _size(dt) + 3) // 4
        nw = (nw + 7) // 8 * 8
        assert self.top + nw <= self.n, ("arena overflow", self.top, nw)
        a = self.t[:, self.top:self.top + nw]
        self.top += nw
        if dt != F32:
            a = a.bitcast(dt)
        a = a[:, 0:nel]
        if len(shape) == 2:
            a = a.rearrange("p (a b) -> p a b", b=shape[1])
        elif len(shape) == 3:
            a = a.rearrange("p (a b c) -> p a b c", b=shape[1], c=shape[2])
        return a


def I_dma(out, in_):
    return lambda e: e.dma_start(out=out, in_=in_)


def I_dma_nc(out, in_):
    return lambda e: e.dma_start(out=out, in_=in_, allow_slow_non_contiguous=True)


_REGS = {}


def bc_reg(e, v):
    if v not in _REGS:
        _REGS[v] = e.to_reg(v)
    return _REGS[v]


def I_act(out, in_, func, **kw):
    return lambda e: e.activation(out=out, in_=in_, func=func, **kw)


def I_tt(out, a, b, op):
    return lambda e: e.tensor_tensor(out=out, in0=a, in1=b, op=op)


def I_ts(out, a, s1, s2, op0, op1=None, accum=None):
    def f(e):
        kw = {}
        if op1 is not None:
            kw["op1"] = op1
        if accum is not None:
            kw["accum_out"] = accum
        return e.tensor_scalar(out=out, in0=a, scalar1=s1, scalar2=s2, op0=op0, **kw)
    return f


def I_stt(out, a, sc, b, op0, op1, accum=None):
    def f(e):
        kw = {}
        if accum is not None:
            kw["accum_out"] = accum
        return e.scalar_tensor_tensor(out=out, in0=a, scalar=sc, in1=b, op0=op0, op1=op1, **kw)
    return f


def I_copy(out, in_):
    return lambda e: e.tensor_copy(out=out, in_=in_)


def I_acopy(out, in_):
    return lambda e: e.copy(out=out, in_=in_)


def I_memset(ap, v):
    return lambda e: e.memset(ap, v)


def I_recip(out, in_):
    return lambda e: e.reciprocal(out=out, in_=in_)


def I_mm(lst):
    def f(e):
        ins = None
        for (out, lhsT, rhs, st, sp, kw) in lst:
            ins = e.matmul(out, lhsT=lhsT, rhs=rhs, start=st, stop=sp, **kw)
        return ins
    return f


def I_trs(lst):
    def f(e):
        ins = None
        for (out, in_, ident) in lst:
            ins = e.transpose(out=out, in_=in_, identity=ident)
        return ins
    return f


DEBUG = []


def build_nc():
    _REGS.clear()
    nc = bass.Bass("TRN2", target_bir_lowering=False)

    def din(name, shape, dt=F32):
        return nc.dram_tensor(name, shape, dt, kind="ExternalInput").ap()

    def dint(name, shape, dt):
        kind = "ExternalOutput" if name in DEBUG else "Internal"
        return nc.dram_tensor(name, shape, dt, kind=kind).ap()

    x_d = din("x", [SEQ, D])
    meta_d = din("meta", [NMETA, D])
    g1_d = din("g1b", [128, D])
    g2_d = din("g2b", [128, D])
    g3_d = din("g3b", [128, D])
    win_d = din("w_in", [D, 3072])
    wout_d = din("w_out", [D, D])
    wr_d = din("w_router", [D, NE])
    wg_d = din("w_gate", [NE, D, DFF])
    wu_d = din("w_up", [NE, D, DFF])
    wd_d = din("w_down", [NE, DFF, D])
    convw_d = din("convw", [128, 4, 3])
    lamp_d = din("lamp", [128, 4, 64])
    gsub_d = din("gsub", [128, 128])
    ropeq_d = din("ropeq", [LP, 128])
    ropek_d = din("ropek", [LP, 128])
    ident_d = din("ident", [128, 128])
    tokid_d = din("tokid", [128, NT])
    dummy_d = din("dummyrows", [128, NCT, 2])
    gmat_d = din("gmat", [128, 128])
    tmat_d = din("tmat", [128, 128])
    out_d = nc.dram_tensor("out", [SEQ, D], F32, kind="ExternalOutput").ap()

    qT_d = dint("qT_s", [4, 128, LP], BF16)
    kT_d = dint("kT_s", [4, 128, LP], BF16)
    v_d = dint("v_s", [4, 128, NT, 128], BF16)
    uT_d = dint("uT_s", [4, 128, L + 2], F32)
    cbT_d = dint("cbT_s", [4, 128, LP], F32)
    attnT_d = dint("attnT_s", [4, 128, LP], BF16)
    convT_d = dint("convT_s", [4, 128, LP], BF16)
    aff_d = dint("aff_s", [NE, L], F32)
    hacc_d = dint("hacc_s", [ROWS, D], F32)
    hs_d = dint("hs_s", [ROWS, D], BF16)
    comb_d = [dint("comb_s%d" % e, [CAPP, 2], F32) for e in range(NE)]
    h1dbg_d = dint("h1dbg_s", [LP, D], F32) if "h1dbg_s" in DEBUG else None

    es = contextlib.ExitStack()
    with es:
        arena_t = es.enter_context(nc.sbuf_tensor("arena", [128, ARENA_WORDS], F32))
        ps = es.enter_context(nc.psum_tensor("ps", [128, 4096], F32))
        p = Prog(nc, es)
        ar = Arena(arena_t, ARENA_WORDS)

        def bank(b, n=1):
            return ps[:, b * 512:(b + n) * 512]

        ident_f = ar.alloc([128])
        ident_b = ar.alloc([128], BF16)
        cols = ar.alloc([64])
        lam_col = cols[:, 0:1]
        zt = ar.alloc([1024])
        misc_ld = p.new_dsem()
        misc_st = p.new_dsem()
        b_ident = Buf()
        b_lam = Buf()
        b_z = Buf()

        p.load1(ident_f, ident_d, b_ident)
        p.op("dve", I_copy(ident_b, ident_f), reads=[b_ident], writes=[b_ident])
        p.op("pool", I_memset(zt, 0.0), writes=[b_z])

        lamp = ar.alloc([4, 64])
        ljunk = ar.alloc([64])
        b_lp = Buf()
        p.load1(lamp, lamp_d, b_lp)
        p.op("dve", I_stt(ljunk, lamp[:, 0, :], 1.0, lamp[:, 1, :], ALU.mult, ALU.mult, accum=cols[:, 1:2]),
             reads=[b_lp], writes=[b_lam])
        p.op("dve", I_stt(ljunk, lamp[:, 2, :], 1.0, lamp[:, 3, :], ALU.mult, ALU.mult, accum=cols[:, 2:3]),
             reads=[b_lp, b_lam], writes=[b_lam])
        p.op("act", I_act(cols[:, 3:5], cols[:, 1:3], AF.Exp), reads=[b_lam], writes=[b_lam])
        p.op("dve", I_tt(cols[:, 5:6], cols[:, 3:4], cols[:, 4:5], ALU.subtract), reads=[b_lam], writes=[b_lam])
        p.op("dve", I_ts(lam_col, cols[:, 5:6], float(LAM_INIT), None, ALU.add), reads=[b_lam], writes=[b_lam])

        dm = ar.alloc([NCT, 2])
        comb = ar.alloc([NT, NE, 2])
        slot_i = ar.alloc([NT, NE], I32)
        b_dm = Buf()

        def emit_scratch_init():
            p.dma("sp", I_dma_nc(uT_d[:, :, 0:1].rearrange("j p o -> p j o"), zt[:, 0:4].unsqueeze(2)), misc_st,
                  reads=[b_z])
            p.dma("sp", I_dma_nc(uT_d[:, :, L + 1:L + 2].rearrange("j p o -> p j o"), zt[:, 0:4].unsqueeze(2)),
                  misc_st, reads=[b_z])
            p.dma("sp", I_dma(hs_d[LP:ROWS, :], zt[0:16, 0:512].bitcast(BF16)), misc_st, reads=[b_z])
            p.dma("sp", I_dma(hacc_d[LP:ROWS, :], zt[0:16, :]), misc_st, reads=[b_z])
            p.load1(dm, dummy_d, b_dm)
            for e_ in range(NE):
                p.dma("sp", I_dma(comb_d[e_].rearrange("(ct p) t -> p ct t", p=128), dm), misc_st, reads=[b_dm])

        persist_top = ar.top

        def rstd_chain(ss_ap, n, tmp_ap, out_ap, b_ss, b_tmp, b_out):
            p.op("dve", I_ts(tmp_ap, ss_ap, 1.0 / n, EPS, ALU.mult, ALU.add), reads=[b_ss], writes=[b_tmp])
            p.op("act", I_act(tmp_ap, tmp_ap, AF.Ln), reads=[], writes=[b_tmp])
            p.op("act", I_act(out_ap, tmp_ap, AF.Exp, scale=-0.5), reads=[b_tmp], writes=[b_out])

        win_sb = ar.alloc([8, 3072], BF16)
        g1b = ar.alloc([1024])
        b_win = Buf()
        b_g = Buf()
        wsem = p.new_dsem()
        winv = win_d.rearrange("(c p) n -> p c n", p=128)
        b_winf = Buf()
        wsemf = p.new_dsem()
        for c in range(8):
            p.dma("pool", I_dma(win_sb[:, c, 1536:3072], winv[:, c, 1536:3072]), wsemf)
        b_winf.w = [(wsemf, wsemf.total)]
        for c in range(8):
            p.dma("pool", I_dma(win_sb[:, c, 0:1536], winv[:, c, 0:1536]), wsem)
        b_win.w = [(wsem, wsem.total)]
        p.load1(g1b, g1_d, b_g)

        xt = [ar.alloc([4, 1024]) for _ in range(2)]
        rtq = ar.alloc([4, 128])
        rtk = ar.alloc([4, 128])
        c1 = ar.alloc([16])
        junkb = ar.alloc([1024], BF16)
        hn = ar.alloc([4, 1024], BF16)
        hnT = [ar.alloc([8, 512], BF16) for _ in range(2)]
        cx_sb = ar.alloc([512])
        uT_st = ar.alloc([4, 512])
        cbT_st = ar.alloc([4, 512])
        rt1 = ar.alloc([8, 64])
        rt2 = ar.alloc([8, 64])
        qk_tm = ar.alloc([4, 2, 512], BF16)
        v_st = ar.alloc([4, 512], BF16)
        qkT_st = ar.alloc([8, 512], BF16)

        b_xt = [Buf(), Buf()]
        b_rt = Buf()
        b_ss = Buf(); b_tmp = Buf(); b_rstd = Buf()
        b_junk = Buf()
        b_hn = [Buf() for _ in range(4)]
        b_hnT = [[Buf() for _ in range(4)] for _ in range(2)]
        b_cx = Buf(); b_uT = Buf(); b_cbT = Buf()
        b_r1 = Buf(); b_r2 = Buf()
        b_qktm = [[Buf() for _ in range(2)] for _ in range(4)]
        b_vst = Buf(); b_qkT = Buf()
        b_ps = [Buf() for _ in range(8)]
        xsem = [p.new_dsem(), p.new_dsem()]
        rsem = p.new_dsem()
        st_u = p.new_dsem(); st_cb = p.new_dsem(); st_qk = p.new_dsem(); st_v = p.new_dsem()

        def load_x(T, bufs, xtile, sems, bxt):
            nsub = 4 if T < 16 else 1
            n0 = T * 512
            if T < 16:
                p.dma("sp", I_dma(xtile[T % 2], x_d[n0:n0 + 512, :].rearrange("(s p) d -> p s d", p=128)),
                      sems[T % 2], writes=[bxt[T % 2]])
            else:
                tokm = p.op("pool", I_memset(xtile[T % 2][:, 0, :], 0.0), writes=[bxt[T % 2]])
                tokd = p.dma("sp", I_dma(xtile[T % 2][0:16, 0, :], meta_d), sems[T % 2], extra=[tokm])
                bxt[T % 2].w.append(tokd)

        NSUP = 17
        rtq2 = [rtq, ar.alloc([4, 128])]
        rtk2 = [rtk, ar.alloc([4, 128])]
        b_rt2 = [Buf(), Buf()]
        rsem2 = [p.new_dsem(), p.new_dsem()]

        def stageA(T):
            nsub = 4 if T < 16 else 1
            N = nsub * 128
            n0 = T * 512
            X = xt[T % 2]
            bX = b_xt[T % 2]
            HT = hnT[T % 2]
            bHT = b_hnT[T % 2]
            p.dma("sp", I_dma(rtq2[T % 2][:, 0:nsub, :], ropeq_d[n0:n0 + N, :].rearrange("(s p) f -> p s f", p=128)),
                  rsem2[T % 2], writes=[b_rt2[T % 2]])
            tk_ = p.dma("sp", I_dma(rtk2[T % 2][:, 0:nsub, :],
                                    ropek_d[n0:n0 + N, :].rearrange("(s p) f -> p s f", p=128)), rsem2[T % 2])
            b_rt2[T % 2].w.append(tk_)
            for s in range(nsub):
                p.op("act", I_act(junkb, X[:, s, :], AF.Square, accum_out=c1[:, s:s + 1]),
                     reads=[bX], writes=[b_junk, b_ss] if s == 0 else [b_junk])
            b_ss.w = [("act", p.cnt["act"])]
            rstd_chain(c1[:, 0:nsub], float(D), c1[:, 4:4 + nsub], c1[:, 8:8 + nsub], b_ss, b_tmp, b_rstd)
            for s in range(nsub):
                p.op("dve", I_stt(hn[:, s, :], X[:, s, :], c1[:, 8 + s:9 + s], g1b, ALU.mult, ALU.mult),
                     reads=[bX, b_rstd, b_g], writes=[b_hn[s]])
            for s in range(nsub):
                pb = bank(s % 2).bitcast(BF16).rearrange("p (c t) -> p c t", t=128)
                p.op("pe", I_trs([(pb[:, c, :], hn[:, s, c * 128:(c + 1) * 128], ident_b) for c in range(8)]),
                     reads=[b_hn[s], b_ident], writes=[b_ps[s % 2]])
                p.op("act" if s % 2 == 0 else "dve",
                     (I_acopy if s % 2 == 0 else I_copy)(HT[:, :, s * 128:(s + 1) * 128], pb),
                     reads=[b_ps[s % 2]], writes=[bHT[s]])

        def chunksB(T):
            nsub = 4 if T < 16 else 1
            N = nsub * 128
            n0 = T * 512
            HT = hnT[T % 2]
            bHT = b_hnT[T % 2]
            rHT = [bHT[s] for s in range(nsub)]
            rtqT = rtq2[T % 2]
            rtkT = rtk2[T % 2]
            bRT = b_rt2[T % 2]
            out = []
            fmc = [0]

            def fm_chunk(j):
                def fmm(col0, pbk):
                    return I_mm([(pbk[:, 0:N], win_sb[:, c, col0:col0 + 128], HT[:, c, 0:N], c == 0, c == 7, {})
                                 for c in range(8)])
                pcx = bank(2 + fmc[0] % 2); bcx = b_ps[2 + fmc[0] % 2]; fmc[0] += 1
                p.op("pe", fmm(1536 + j * 128, pcx), reads=rHT + [b_winf], writes=[bcx])
                p.op("act", I_acopy(cx_sb[:, 0:N], pcx[:, 0:N]), reads=[bcx], writes=[b_cx])
                pcc = bank(2 + fmc[0] % 2); bcc = b_ps[2 + fmc[0] % 2]; fmc[0] += 1
                p.op("pe", fmm(2560 + j * 128, pcc), reads=rHT + [b_winf], writes=[bcc])
                p.op("dve", I_tt(uT_st[:, j, 0:N], pcc[:, 0:N], cx_sb[:, 0:N], ALU.mult),
                     reads=[bcc, b_cx], writes=[b_uT] if j == 0 else [])
                pcb = bank(2 + fmc[0] % 2); bcb = b_ps[2 + fmc[0] % 2]; fmc[0] += 1
                p.op("pe", fmm(2048 + j * 128, pcb), reads=rHT + [b_winf], writes=[bcb])
                p.op("act", I_acopy(cbT_st[:, j, 0:N], pcb[:, 0:N]), reads=[bcb], writes=[b_cbT] if j == 0 else [])
                if j == 3:
                    b_uT.w = [("dve", p.cnt["dve"])]
                    b_cbT.w = [("act", p.cnt["act"])]
                    NV = N if T < 16 else NMETA
                    ucol = (17 + n0) if T < 16 else 1
                    p.dma("pool", I_dma(uT_d[:, :, ucol:ucol + NV].rearrange("j p n -> p j n"), uT_st[:, :, 0:NV]),
                          st_u, reads=[b_uT])
                    p.dma("pool", I_dma(cbT_d[:, :, n0:n0 + N].rearrange("j p n -> p j n"), cbT_st[:, :, 0:N]),
                          st_cb, reads=[b_cbT])

            for j in range(4):
                out.append(lambda j=j: fm_chunk(j))

            tmc = [0]
            deferred = []
            state = {"first_qk": True, "first_v": True}

            def qk_transposes(s, grp):
                pq = bank(6 + (s * 2 + grp) % 2).bitcast(BF16)[:, 0:512].rearrange("p (h t) -> p h t", t=128)
                bpq = b_ps[6 + (s * 2 + grp) % 2]
                p.op("pe", I_trs([(pq[:, h, :], qk_tm[:, s, grp, h * 128:(h + 1) * 128], ident_b)
                                  for h in range(4)]),
                     reads=[b_qktm[s][grp], b_ident], writes=[bpq])
                p.op("act", I_acopy(qkT_st[:, grp * 4:(grp + 1) * 4, s * 128:(s + 1) * 128], pq),
                     reads=[bpq], writes=[b_qkT] if state["first_qk"] else [])
                state["first_qk"] = False

            def tm_chunk(s, grp):
                pt = bank(4 + tmc[0] % 2); bpt = b_ps[4 + tmc[0] % 2]; tmc[0] += 1
                p.op("pe", I_mm([(pt, HT[:, c, s * 128:(s + 1) * 128], win_sb[:, c, grp * 512:(grp + 1) * 512],
                                  c == 0, c == 7, {}) for c in range(8)]),
                     reads=[bHT[s], b_win], writes=[bpt])
                if grp == 2:
                    p.op("act", I_acopy(v_st[:, s, :], pt), reads=[bpt], writes=[b_vst] if state["first_v"] else [])
                    state["first_v"] = False
                else:
                    rt = rtqT if grp == 0 else rtkT
                    psq = pt.rearrange("p (g d) -> p g d", d=64)
                    c2b = rt[:, s, 0:64].unsqueeze(1).to_broadcast([128, 8, 64])
                    nsb = rt[:, s, 64:96].unsqueeze(1).to_broadcast([128, 8, 32])
                    psb = rt[:, s, 96:128].unsqueeze(1).to_broadcast([128, 8, 32])
                    p.op("dve", I_tt(rt1, psq, c2b, ALU.mult), reads=[bpt, bRT], writes=[b_r1])
                    p.op("dve", I_tt(rt2[:, :, 0:32], psq[:, :, 32:64], nsb, ALU.mult), reads=[bpt, bRT],
                         writes=[b_r2])
                    tk2 = p.op("dve", I_tt(rt2[:, :, 32:64], psq[:, :, 0:32], psb, ALU.mult), reads=[bpt, bRT])
                    b_r2.w.append(tk2)
                    dst = qk_tm[:, s, grp, :].rearrange("p (g d) -> p g d", d=64)
                    p.op("dve", I_tt(dst, rt1, rt2, ALU.add), reads=[b_r1, b_r2], writes=[b_qktm[s][grp]])
                    deferred.append((s, grp))
                while len(deferred) > 2:
                    qk_transposes(*deferred.pop(0))

            for s in range(nsub):
                for grp in range(3):
                    out.append(lambda s=s, grp=grp: tm_chunk(s, grp))

            def finish():
                while deferred:
                    qk_transposes(*deferred.pop(0))
                b_qkT.w = [("act", p.cnt["act"])]
                b_vst.w = [("act", p.cnt["act"])]
                p.dma("pool", I_dma(qT_d[:, :, n0:n0 + N].rearrange("h p n -> p h n"), qkT_st[:, 0:4, 0:N]), st_qk,
                      reads=[b_qkT])
                p.dma("pool", I_dma(kT_d[:, :, n0:n0 + N].rearrange("h p n -> p h n"), qkT_st[:, 4:8, 0:N]), st_qk,
                      reads=[b_qkT])
                for h in range(4):
                    p.dma("pool", I_dma(v_d[h, :, T * 4:T * 4 + nsub, :], v_st[:, 0:nsub, h * 128:(h + 1) * 128]),
                          st_v, reads=[b_vst])
            out.append(finish)
            return out

        load_x(0, None, xt, xsem, b_xt)
        stageA(0)
        for T in range(NSUP):
            if T + 1 < NSUP:
                load_x(T + 1, None, xt, xsem, b_xt)
            ch = chunksB(T)
            half = 5
            for f_ in ch[:half]:
                f_()
            if T + 1 < NSUP:
                stageA(T + 1)
            for f_ in ch[half:]:
                f_()
        p.barrier()
        ar.top = persist_top

        QT = [ar.alloc([LP], BF16) for _ in range(2)]
        KT = [ar.alloc([LP], BF16) for _ in range(2)]
        VA = [ar.alloc([NT, 129], BF16) for _ in range(2)]
        Eb = [ar.alloc([2, 512], BF16) for _ in range(3)]
        gsub_b = ar.alloc([128])
        tmp_t = ar.alloc([128])
        o_s = ar.alloc([4, 128])
        on_s = ar.alloc([4, 128], BF16)
        jk = ar.alloc([128])
        c2 = ar.alloc([32])
        accs = ar.alloc([3, 408])
        aT_st = [ar.alloc([512], BF16) for _ in range(2)]
        b_qkv = [Buf(), Buf()]
        b_E = [Buf() for _ in range(3)]
        b_S = [Buf(), Buf()]
        b_acc = Buf()
        b_gs = Buf()
        b_c2 = Buf(); b_tt = Buf(); b_o = Buf(); b_on = Buf(); b_jk = Buf(); b_ms = Buf(); b_ms2 = Buf(); b_rs = Buf()
        b_accs = Buf()
        b_p7 = Buf()
        b_aT = [Buf(), Buf()]
        hsem = [p.new_dsem(), p.new_dsem()]
        st2 = [p.new_dsem(), p.new_dsem()]

        p.load1(gsub_b, gsub_d, b_gs)
        p.op("dve", I_ts(gsub_b, gsub_b, float(1.0 - LAM_INIT), None, ALU.mult), reads=[], writes=[b_gs])

        convw = ar.alloc([4, 3])
        cut = [ar.alloc([4, 514]) for _ in range(2)]
        ccb = [ar.alloc([4, 512]) for _ in range(2)]
        cca = ar.alloc([512])
        ccb2 = ar.alloc([512])
        cv_st = [ar.alloc([4, 512], BF16) for _ in range(2)]
        b_cw = Buf(); b_cut = [Buf(), Buf()]; b_ccb = [Buf(), Buf()]; b_cca = Buf(); b_ccb2 = Buf()
        b_cv = [Buf(), Buf()]
        ld_cut = [p.new_dsem(), p.new_dsem()]
        ld_ccb = [p.new_dsem(), p.new_dsem()]
        st_cv = [p.new_dsem(), p.new_dsem()]
        p.load1(convw, convw_d, b_cw)
        p.op("dve", I_memset(cv_st[0], 0.0), writes=[b_cv[0]])
        p.op("dve", I_memset(cv_st[1], 0.0), writes=[b_cv[1]])

        def conv_load(T):
            nsub = 4 if T < 16 else 1
            N = nsub * 128
            n0 = T * 512
            NV = N if T < 16 else NMETA
            sl = T % 2
            ucol = (16 + n0) if T < 16 else 0
            p.dma("sp", I_dma(cut[sl][:, :, 0:NV + 2], uT_d[:, :, ucol:ucol + NV + 2].rearrange("j p n -> p j n")),
                  ld_cut[sl], writes=[b_cut[sl]])
            p.dma("sp", I_dma(ccb[sl][:, :, 0:N], cbT_d[:, :, n0:n0 + N].rearrange("j p n -> p j n")), ld_ccb[sl],
                  writes=[b_ccb[sl]])

        def conv_compute(T):
            nsub = 4 if T < 16 else 1
            N = nsub * 128
            n0 = T * 512
            NV = N if T < 16 else NMETA
            sl = T % 2
            U = cut[sl]
            for j in range(4):
                p.op("dve", I_ts(cca[:, 0:NV], U[:, j, 0:NV], convw[:, j, 0:1], None, ALU.mult),
                     reads=[b_cut[sl], b_cw], writes=[b_cca])
                p.op("dve", I_stt(ccb2[:, 0:NV], U[:, j, 1:NV + 1], convw[:, j, 1:2], cca[:, 0:NV], ALU.mult, ALU.add),
                     reads=[b_cut[sl], b_cca], writes=[b_ccb2])
                p.op("dve", I_stt(cca[:, 0:NV], U[:, j, 2:NV + 2], convw[:, j, 2:3], ccb2[:, 0:NV], ALU.mult, ALU.add),
                     reads=[b_cut[sl], b_ccb2], writes=[b_cca])
                p.op("dve", I_tt(cv_st[sl][:, j, 0:NV], cca[:, 0:NV], ccb[sl][:, j, 0:NV], ALU.mult),
                     reads=[b_cca, b_ccb[sl]], writes=[b_cv[sl]] if j == 0 else [])
            b_cv[sl].w = [("dve", p.cnt["dve"])]
            p.dma("pool", I_dma(convT_d[:, :, n0:n0 + N].rearrange("j p n -> p j n"), cv_st[sl][:, :, 0:N]), st_cv[sl],
                  reads=[b_cv[sl]])
        b_va1 = [Buf(), Buf()]
        for sl in range(2):
            p.op("pool", I_memset(VA[sl][:, :, 128:129], 1.0), writes=[b_va1[sl]])
            p.op("pool", I_memset(VA[sl][:, NT - 1, 128:129], 0.0), writes=[b_va1[sl]])
            p.op("pool", I_memset(VA[sl][0:16, NT - 1, 128:129], 1.0), writes=[b_va1[sl]])

        def load_head(h):
            sl = h % 2
            p.dma("sp", I_dma(QT[sl], qT_d[h]), hsem[sl], writes=[b_qkv[sl]])
            t1_ = p.dma("sp", I_dma(KT[sl], kT_d[h]), hsem[sl])
            t2_ = p.dma("sp", I_dma(VA[sl][:, :, 0:128], v_d[h]), hsem[sl])
            b_qkv[sl].w += [t1_, t2_]

        def S2(sb):
            return bank(2 * sb, 2).rearrange("p (a c) -> p a c", c=512)

        def acc_ap(i, s, nsub=4):
            a = i * nsub + s
            bk = bank(4 + a // 3)
            o = (a % 3) * 129
            return bk[:, o:o + 129]

        items = [(h, qb, kt) for h in range(4) for qb in range(17) for kt in range(NT)]

        Qp = [ar.alloc([2, 512], BF16) for _ in range(2)]
        b_qp = [Buf(), Buf()]
        for sl_ in range(2):
            p.op("dve", I_memset(Qp[sl_], 0.0), writes=[b_qp[sl_]])

        def emit_qpad(h, qb):
            sl = h % 2
            qs = (h * 17 + qb) % 2
            q0 = qb * 512
            nq = 512 if qb < 16 else 128
            p.op("dve", I_copy(Qp[qs][0:64, 0, 0:nq], QT[sl][0:64, q0:q0 + nq]), reads=[b_qkv[sl]],
                 writes=[b_qp[qs]])
            tk_ = p.op("dve", I_copy(Qp[qs][64:128, 1, 0:nq], QT[sl][64:128, q0:q0 + nq]), reads=[b_qkv[sl]])
            b_qp[qs].w.append(tk_)

        def emit_qk(idx):
            h, qb, kt = items[idx]
            sl = h % 2
            sb = idx % 2
            qs = (h * 17 + qb) % 2
            nq = 512 if qb < 16 else 128
            s2 = S2(sb)
            p.op("pe", I_mm([
                (s2[:, 0, 0:nq], KT[sl][:, kt * 128:(kt + 1) * 128], Qp[qs][:, 0, 0:nq], True, True, {}),
                (s2[:, 1, 0:nq], KT[sl][:, kt * 128:(kt + 1) * 128], Qp[qs][:, 1, 0:nq], True, True, {})]),
                 reads=[b_qkv[sl], b_qp[qs]], writes=[b_S[sb]])

        def emit_exp_av(idx, nxt=None):
            h, qb, kt = items[idx]
            sl = h % 2
            sb = idx % 2
            eb = idx % 3
            nq = 512 if qb < 16 else 128
            nsub = nq // 128
            na = nq if qb < 16 else NMETA
            p.op("act", I_act(Eb[eb][:, :, 0:na], S2(sb)[:, :, 0:na], AF.Exp), reads=[b_S[sb]], writes=[b_E[eb]])
            if nxt is not None:
                emit_qk(nxt)
            lst = []
            seen = set()
            for i in range(2):
                for s in range(nsub):
                    bk_ = (i * nsub + s) // 3
                    st_ = (kt == 0) and (bk_ not in seen)
                    seen.add(bk_)
                    lst.append((acc_ap(i, s, nsub), Eb[eb][:, i, s * 128:(s + 1) * 128], VA[sl][:, kt, :],
                                st_, kt == NT - 1, dict(skip_group_check=True)))
            extra = list(b_acc.r) if kt == 0 else []
            tok = p.op("pe", I_mm(lst), reads=[b_E[eb], b_qkv[sl], b_va1[sl]], extra=extra)
            if kt == NT - 1:
                b_acc.w = [tok]
                b_acc.r = []

        epi_cnt = [0]
        pending = []

        def acc_sb(i, s_, nsub=4):
            a_ = i * nsub + s_
            o_ = (a_ % 3) * 129
            return accs[:, a_ // 3, o_:o_ + 129]

        def emit_epilogue(h, qb, idx):
            q0 = qb * 512
            nq = 512 if qb < 16 else 128
            nsub = nq // 128
            sts = epi_cnt[0] % 2
            epi_cnt[0] += 1
            nacc = 2 * nsub
            toksA = []
            for bk in range((nacc + 2) // 3):
                w_ = 129 * min(3, nacc - 3 * bk)
                toksA.append(p.op("dve", I_copy(accs[:, bk, 0:w_], bank(4 + bk)[:, 0:w_]), reads=[b_acc],
                                  writes=[b_accs] if bk == 0 else []))
            b_accs.w = toksA
            for s_ in range(nsub):
                a0 = acc_sb(0, s_, nsub)
                a1 = acc_sb(1, s_, nsub)
                p.op("dve", I_recip(c2[:, 0:1], a0[:, 128:129]), reads=[b_accs], writes=[b_c2])
                p.op("dve", I_recip(c2[:, 1:2], a1[:, 128:129]), reads=[b_accs], writes=[b_c2])
                p.op("dve", I_tt(c2[:, 2:3], c2[:, 1:2], lam_col, ALU.mult), reads=[b_lam], writes=[b_c2])
                p.op("dve", I_ts(tmp_t, a1[:, 0:128], c2[:, 2:3], None, ALU.mult), reads=[b_accs, b_c2],
                     writes=[b_tt])
                p.op("dve", I_stt(o_s[:, s_, :], a0[:, 0:128], c2[:, 0:1], tmp_t, ALU.mult, ALU.subtract),
                     reads=[b_accs, b_c2, b_tt], writes=[b_o])
                p.op("dve", I_stt(jk, o_s[:, s_, :], 1.0, o_s[:, s_, :], ALU.mult, ALU.mult,
                                  accum=c2[:, 8 + s_:9 + s_]), reads=[b_o], writes=[b_jk, b_ms])
            p.op("dve", I_ts(c2[:, 12:12 + nsub], c2[:, 8:8 + nsub], 1.0 / 128.0, EPS, ALU.mult, ALU.add),
                 reads=[b_ms], writes=[b_ms2])

            def stage_cd():
                p.op("act", I_act(c2[:, 12:12 + nsub], c2[:, 12:12 + nsub], AF.Ln), writes=[b_ms2])
                p.op("act", I_act(c2[:, 16:16 + nsub], c2[:, 12:12 + nsub], AF.Exp, scale=-0.5), reads=[b_ms2],
                     writes=[b_rs])
                for s_ in range(nsub):
                    p.op("dve", I_stt(on_s[:, s_, :], o_s[:, s_, :], c2[:, 16 + s_:17 + s_], gsub_b, ALU.mult,
                                      ALU.mult), reads=[b_o, b_rs, b_gs], writes=[b_on])

            def stage_e():
                p7 = bank(7).bitcast(BF16)[:, 0:512].rearrange("p (s t) -> p s t", t=128)
                p.op("pe", I_trs([(p7[:, s_, :], on_s[:, s_, :], ident_b) for s_ in range(nsub)]),
                     reads=[b_on, b_ident], writes=[b_p7])
                p.op("dve", I_copy(aT_st[sts][:, 0:nq], bank(7).bitcast(BF16)[:, 0:nq]), reads=[b_p7],
                     writes=[b_aT[sts]])
                p.dma("pool", I_dma(attnT_d[h, :, q0:q0 + nq], aT_st[sts][:, 0:nq]), st2[sts], reads=[b_aT[sts]])

            pending.append((idx + 14, stage_cd))
            pending.append((idx + 26, stage_e))

        load_head(0)
        load_head(1)
        conv_load(0)
        emit_scratch_init()
        emit_qpad(0, 0)
        emit_qk(0)
        emit_qk(1)
        for idx in range(len(items)):
            h, qb, kt = items[idx]
            if kt == 0 and idx + NT < len(items):
                nh, nqb, _ = items[idx + NT]
                emit_qpad(nh, nqb)
            emit_exp_av(idx, idx + 2 if idx + 2 < len(items) else None)
            if h == 0 and kt == 5 and qb + 1 < NSUP:
                conv_load(qb + 1)
            if h == 0 and kt == 30:
                conv_compute(qb)
            while pending and pending[0][0] <= idx:
                pending.pop(0)[1]()
            if kt == NT - 1:
                emit_epilogue(h, qb, idx)
                if qb == 16 and h + 2 < 4:
                    load_head(h + 2)
        while pending:
            pending.pop(0)[1]()
        p.barrier()
        ar.top = persist_top

        wout_sb = ar.alloc([8, 1024], BF16)
        wr_sb = ar.alloc([8, 16])
        wr_hi = ar.alloc([8, 16], BF16)
        wr_lo = ar.alloc([8, 16], BF16)
        g2b = ar.alloc([1024])
        affT = ar.alloc([LP])
        tokid = ar.alloc([NT])
        p34_top = ar.top
        mixT = [ar.alloc([8, 512], BF16) for _ in range(2)]
        xt3 = [ar.alloc([4, 1024]) for _ in range(2)]
        h1 = [ar.alloc([1024]) for _ in range(4)]
        hsf = [ar.alloc([1024]) for _ in range(2)]
        hhi = [ar.alloc([1024], BF16) for _ in range(2)]
        hlo = [ar.alloc([1024], BF16) for _ in range(2)]
        hlT = [ar.alloc([16, 128], BF16) for _ in range(2)]
        c3 = ar.alloc([64])
        sqjunk = ar.alloc([1024], BF16)
        b_sqj = Buf()
        ex = [ar.alloc([16]) for _ in range(2)]

        b_wout = Buf(); b_wr = Buf(); b_g2 = Buf(); b_cw = Buf(); b_tok = Buf(); b_comb = Buf(); b_affT = Buf()
        b_mix = [Buf(), Buf()]
        b_mixc = [[Buf() for _ in range(4)] for _ in range(2)]
        b_ut = [Buf(), Buf()]; b_cbt = [Buf(), Buf()]; b_ca = Buf(); b_cb2 = Buf()
        b_xt3 = [Buf(), Buf()]
        b_h1 = [Buf() for _ in range(4)]
        b_hsf = [Buf(), Buf()]; b_hhi = [Buf(), Buf()]; b_hlo = [Buf(), Buf()]; b_hlT = [Buf(), Buf()]
        b_ss3 = [Buf(), Buf()]; b_t3 = [Buf(), Buf()]; b_r3 = [Buf(), Buf()]
        b_lg = [Buf(), Buf()]; b_ex = [Buf(), Buf()]; b_se = [Buf(), Buf()]
        b_po = [Buf(), Buf()]; b_pT = Buf(); b_plg = [Buf(), Buf()]; b_paT = [Buf(), Buf()]
        wsem3 = p.new_dsem()
        ld3 = [p.new_dsem(), p.new_dsem()]
        ld_ut = [p.new_dsem(), p.new_dsem()]
        ld_cbt = [p.new_dsem(), p.new_dsem()]
        st_h1 = [p.new_dsem() for _ in range(4)]
        st_hs = [p.new_dsem(), p.new_dsem()]
        xsem3 = [p.new_dsem(), p.new_dsem()]

        woutv = wout_d.rearrange("(c p) n -> p c n", p=128)
        for c in range(8):
            p.dma("pool", I_dma(wout_sb[:, c, :], woutv[:, c, :]), wsem3)
        b_wout.w = [(wsem3, wsem3.total)]
        p.load1(wr_sb, wr_d.rearrange("(c p) e -> p c e", p=128), b_wr)
        b_wrh = Buf()
        p.op("dve", I_copy(wr_hi, wr_sb), reads=[b_wr], writes=[b_wrh])
        p.op("dve", I_tt(wr_lo, wr_sb, wr_hi, ALU.subtract), reads=[b_wr, b_wrh], writes=[])
        b_wr.w = [("dve", p.cnt["dve"])]
        p.load1(g2b, g2_d, b_g2)
        p.load1(tokid, tokid_d, b_tok)
        p.op("dve", I_copy(comb[:, :, :, 0], tokid.unsqueeze(2).to_broadcast([128, NT, NE])), reads=[b_tok],
             writes=[b_comb])

        def sup(T):
            nsub = 4 if T < 16 else 1
            return nsub, nsub * 128, T * 512, (nsub * 128 if T < 16 else NMETA)

        def load3(T):
            nsub, N, n0, NV = sup(T)
            sl = T % 2
            p.dma("sp", I_dma(mixT[sl][:, 0:4, 0:N], attnT_d[:, :, n0:n0 + N].rearrange("h p n -> p h n")), ld3[sl],
                  writes=[b_mix[sl]])
            load_x(T, None, xt3, xsem3, b_xt3)
            p.dma("sp", I_dma(mixT[sl][:, 4:8, 0:N], convT_d[:, :, n0:n0 + N].rearrange("j p n -> p j n")), ld3[sl])
            b_mix[sl].w.append((ld3[sl], ld3[sl].total))

        subs = []
        for T in range(NSUP):
            for s in range(4 if T < 16 else 1):
                subs.append((T, s))
        NJ = len(subs)

        def S0(j):
            T, s = subs[j]
            sl = T % 2
            MT = mixT[sl]
            po = bank(2 * (j % 2), 2)
            lst = []
            for half in range(2):
                for m in range(8):
                    lst.append((po[:, half * 512:(half + 1) * 512], MT[:, m, s * 128:(s + 1) * 128],
                                wout_sb[:, m, half * 512:(half + 1) * 512], m == 0, m == 7, {}))
            p.op("pe", I_mm(lst), reads=[b_mix[sl], b_wout] + b_mixc[sl], writes=[b_po[j % 2]])

        def S1(j):
            T, s = subs[j]
            sl = T % 2
            hb = j % 2
            r0 = T * 512 + s * 128
            h4 = j % 4
            p.op("dve", I_tt(h1[h4], bank(2 * hb, 2), xt3[sl][:, s, :], ALU.add), reads=[b_po[hb], b_xt3[sl]],
                 writes=[b_h1[h4]])
            p.dma("pool", I_dma(hacc_d[r0:r0 + 128, :], h1[h4]), st_h1[h4], reads=[b_h1[h4]])
            if h1dbg_d is not None:
                p.dma("pool", I_dma(h1dbg_d[r0:r0 + 128, :], h1[h4]), st_h1[h4], reads=[b_h1[h4]])
            p.op("act", I_act(sqjunk, h1[h4], AF.Square, accum_out=c3[:, hb:hb + 1]), reads=[b_h1[h4]],
                 writes=[b_sqj, b_ss3[hb]])

        def S2(j):
            hb = j % 2
            rstd_chain(c3[:, hb:hb + 1], float(D), c3[:, 2 + hb:3 + hb], c3[:, 4 + hb:5 + hb], b_ss3[hb], b_t3[hb],
                       b_r3[hb])

        def S3a(j):
            T, s = subs[j]
            hb = j % 2
            r0 = T * 512 + s * 128
            p.op("dve", I_stt(hsf[hb], h1[j % 4], c3[:, 4 + hb:5 + hb], g2b, ALU.mult, ALU.mult),
                 reads=[b_h1[j % 4], b_r3[hb], b_g2], writes=[b_hsf[hb]])
            p.op("act", I_acopy(hhi[hb], hsf[hb]), reads=[b_hsf[hb]], writes=[b_hhi[hb]])
            p.dma("pool", I_dma(hs_d[r0:r0 + 128, :], hhi[hb]), st_hs[hb], reads=[b_hhi[hb]])

        def S3b(j):
            hb = j % 2
            p.op("dve", I_tt(hlo[hb], hsf[hb], hhi[hb], ALU.subtract), reads=[b_hsf[hb], b_hhi[hb]],
                 writes=[b_hlo[hb]])

        def S4(j):
            hb = j % 2
            pT = bank(4, 2).bitcast(BF16).rearrange("p (c t) -> p c t", t=128)
            lst = [(pT[:, c, :], hhi[hb][:, c * 128:(c + 1) * 128], ident_b) for c in range(8)]
            lst += [(pT[:, 8 + c, :], hlo[hb][:, c * 128:(c + 1) * 128], ident_b) for c in range(8)]
            p.op("pe", I_trs(lst), reads=[b_hhi[hb], b_hlo[hb], b_ident], writes=[b_pT])
            p.op("act", I_acopy(hlT[hb], pT), reads=[b_pT], writes=[b_hlT[hb]])

        def S5a(j):
            hb = j % 2
            plg = bank(6)[:, hb * 16:(hb + 1) * 16]
            lst = []
            for c in range(8):
                lst.append((plg, hlT[hb][:, c, :], wr_hi[:, c, :], c == 0, False, {}))
                lst.append((plg, hlT[hb][:, c, :], wr_lo[:, c, :], False, False, {}))
                lst.append((plg, hlT[hb][:, 8 + c, :], wr_hi[:, c, :], False, c == 7, {}))
            p.op("pe", I_mm(lst), reads=[b_hlT[hb], b_wr], writes=[b_plg[hb]])
            p.op("dve", lambda e, plg=plg, hb=hb: e.reduce_max(out=c3[:, 8 + hb:9 + hb], in_=plg,
                                                               axis=mybir.AxisListType.X),
                 reads=[b_plg[hb]], writes=[b_lg[hb]])
            p.op("dve", I_ts(c3[:, 10 + hb:11 + hb], c3[:, 8 + hb:9 + hb], -1.0, None, ALU.mult), reads=[b_lg[hb]],
                 writes=[b_lg[hb]])
            p.op("act", I_act(ex[hb], plg, AF.Exp, bias=c3[:, 10 + hb:11 + hb], accum_out=c3[:, 12 + hb:13 + hb]),
                 reads=[b_plg[hb], b_lg[hb]], writes=[b_ex[hb], b_se[hb]])

        def S5b(j):
            hb = j % 2
            p.op("dve", I_recip(c3[:, 14 + hb:15 + hb], c3[:, 12 + hb:13 + hb]), reads=[b_se[hb]], writes=[b_se[hb]])
            p.op("dve", I_ts(comb[:, j, :, 1], ex[hb], c3[:, 14 + hb:15 + hb], None, ALU.mult),
                 reads=[b_ex[hb], b_se[hb]], writes=[b_comb])

        def S6(j):
            hb = j % 2
            paT = bank(7)[0:16, hb * 128:(hb + 1) * 128]
            p.op("pe", I_trs([(paT, comb[:, j, :, 1], ident_f)]), reads=[b_comb, b_ident], writes=[b_paT[hb]])
            p.op("dve", I_copy(affT[0:16, j * 128:(j + 1) * 128], paT), reads=[b_paT[hb]], writes=[])

        stages = [S0, S1, S2, S3a, S3b, S4, S5a, S5b, S6]
        load3(0)
        for t in range(NJ + len(stages) - 1):
            for k in reversed(range(len(stages))):
                j = t - k
                if 0 <= j < NJ:
                    stages[k](j)
            if t < NJ:
                T, s = subs[t]
                if s == 0 and T + 1 < NSUP:
                    load3(T + 1)
        b_affT.w = [("dve", p.cnt["dve"])]
        p.barrier()
        ar.top = p34_top

        ar.top = p34_top
        wslot = [ar.alloc_top([8, 1024], BF16) for _ in range(6)]
        b_ws = [Buf() for _ in range(6)]
        wsem5 = [p.new_dsem() for _ in range(6)]

        def load_w(e_, k):
            if k < 4:
                src = (wg_d if k % 2 == 0 else wu_d)[e_].rearrange("(c p) f -> p c f", p=128)
                fh = k // 2
                src = src[:, :, fh * 1024:(fh + 1) * 1024]
            else:
                fh = k - 4
                src = wd_d[e_].rearrange("(c p) d -> p c d", p=128)[:, fh * 8:(fh + 1) * 8, :]
            for c in range(0, 8, 2):
                p.dma("pool", I_dma(wslot[k][:, c:c + 2, :], src[:, c:c + 2, :]), wsem5[k],
                      writes=[b_ws[k]] if c == 0 else [])
            b_ws[k].w = [(wsem5[k], wsem5[k].total)]

        for k in range(6):
            load_w(0, k)

        SEG = L // 8
        A128 = ar.alloc([SEG])
        msk = ar.alloc([SEG])
        cum = ar.alloc([SEG])
        ones = ar.alloc([SEG])
        gmat = ar.alloc([128])
        tmat = ar.alloc([128])
        c4 = ar.alloc([16])
        slotf = affT
        b4 = Buf()
        b_A = Buf(); b_gm = Buf(); b_tm = Buf(); b_cnt = Buf(); b_tot = Buf(); b_sl = Buf(); b_si = Buf()
        b_p4 = Buf(); b_s128 = Buf()
        bsem = [p.new_dsem() for _ in range(4)]
        p.load1(gmat, gmat_d, b_gm)
        p.load1(tmat, tmat_d, b_tm)
        t_st = p.dma("sp", I_dma(aff_d, affT[0:16, 0:L]), bsem[0], reads=[b_affT])
        p.dma("sp", I_dma(A128, aff_d.rearrange("e (s c) -> (e s) c", c=SEG)), bsem[1], writes=[b_A], extra=[t_st])
        lo = c4[:, 0:1]
        mid = c4[:, 1:2]
        cnt = c4[:, 2:3]
        dl = c4[:, 3:4]
        tot_ps = bank(3)[:, 0:1]
        p.op("dve", I_memset(c4, 0.0), writes=[b4])
        p.op("dve", I_memset(ones, 1.0), writes=[b4])
        junk3 = ar.alloc([SEG])
        w3 = c4[:, 5:8]
        tvec = c4[:, 8:11]
        cnt3 = c4[:, 11:14]
        ssum = c4[:, 14:15]
        ge3 = ar.alloc([8])
        tot3_ps = bank(3)[:, 0:3]
        b_tv = Buf(); b_c3 = Buf(); b_s = Buf(); b_lo = Buf()
        for i_ in range(3):
            p.op("dve", I_memset(c4[:, 5 + i_:6 + i_], float(i_ + 1)), writes=[b4])
        b_lo.w = list(b4.w)
        jbufs = [msk, cum, junk3]
        for k in range(NBIS // 2):
            w = 4.0 ** -(k + 1)
            p.op("dve", I_ts(tvec, w3, w, lo, ALU.mult, ALU.add), reads=[b_lo], writes=[b_tv])
            toks = []
            for i_ in range(3):
                toks.append(p.op("dve", I_ts(jbufs[i_], A128, tvec[:, i_:i_ + 1], 0.0, ALU.is_ge, ALU.add,
                                             accum=cnt3[:, i_:i_ + 1]),
                                 reads=[b_A, b_tv], writes=[b_c3] if i_ == 0 else []))
            b_c3.w = toks
            p.op("pe", I_mm([(tot3_ps, gmat, cnt3, True, True, {})]), reads=[b_c3, b_gm], writes=[b_tot])
            p.op("dve", I_ts(ge3[:, 0:3], tot3_ps, float(CAP) - 0.5, 0.0, ALU.is_ge, ALU.add, accum=ssum),
                 reads=[b_tot], writes=[b_s])
            p.op("dve", I_stt(lo, ssum, w, lo, ALU.mult, ALU.add), reads=[b_s], writes=[b_lo])
        b4.w = list(b_lo.w) + list(b_c3.w)
        p.op("dve", I_ts(msk, A128, lo, None, ALU.is_ge), reads=[b_A], writes=[b4])
        p.op("dve", lambda e: e.tensor_tensor_scan(out=cum, data0=ones, data1=msk, initial=0.0, op0=ALU.mult,
                                                   op1=ALU.add), writes=[b4, b_cnt])
        p.op("pe", I_mm([(tot_ps, tmat, cum[:, SEG - 1:SEG], True, True, {})]), reads=[b_cnt, b_tm], writes=[b_tot])
        p.op("dve", I_ts(c4[:, 4:5], tot_ps, -(BIG + 1.0), None, ALU.add), reads=[b_tot], writes=[b4])
        p.op("dve", I_stt(cum, cum, c4[:, 4:5], msk, ALU.add, ALU.mult), writes=[b4])
        p.op("dve", I_ts(cum, cum, BIG, None, ALU.add), writes=[b4, b_s128])
        t_st2 = p.dma("sp", I_dma(aff_d.rearrange("e (s c) -> (e s) c", c=SEG), cum), bsem[2], reads=[b_s128],
                      extra=[(bsem[1], bsem[1].total)])
        p.op("dve", I_memset(slotf[0:16, L:LP], BIG), writes=[b4])
        t_ld2 = p.dma("sp", I_dma(slotf[0:16, 0:L], aff_d), bsem[3], extra=[t_st2, (bsem[0], bsem[0].total)])
        b_sl.w = [t_ld2, ("dve", p.cnt["dve"])]
        for g in range(3):
            j0 = g * 32
            j1 = min(NT, j0 + 32)
            pg = bank(g)
            p.op("pe", I_trs([(pg[:, (j - j0) * 16:(j - j0 + 1) * 16], slotf[0:16, j * 128:(j + 1) * 128],
                               ident_f[0:16, 0:16]) for j in range(j0, j1)]),
                 reads=[b_sl, b_ident], writes=[b_p4])
            p.op("dve", I_copy(slot_i[:, j0:j1, :], pg[:, 0:(j1 - j0) * 16].rearrange("p (j e) -> p j e", e=16)),
                 reads=[b_p4], writes=[b_si])
        csem = [p.new_dsem() for _ in range(NE)]

        def emit_comb_scatter(e_, j0=0, j1=NT):
            for j in range(j0, j1):
                def sc(e, e_=e_, j=j):
                    return e.indirect_dma_start(
                        out=comb_d[e_], out_offset=bass.IndirectOffsetOnAxis(ap=slot_i[:, j, e_:e_ + 1], axis=0),
                        in_=comb[:, j, e_, :], in_offset=None, bounds_check=bc_reg(e, CAPP - 1), oob_is_err=False)
                p.dma("pool", sc, csem[e_], reads=[b_si, b_comb])

        emit_comb_scatter(0)
        p.barrier_on([("dve", p.cnt["dve"]), ("pe", p.cnt["pe"]), ("act", p.cnt["act"])])
        ar.top = persist_top

        xg = ar.alloc([NCT, 1024], BF16)
        _xgT = ar.alloc([8, CAPP], BF16)
        xgT = [_xgT, _xgT]
        actT = ar.alloc([16, CAPP], BF16)
        sil = [ar.alloc([512]) for _ in range(2)]
        NY = 4
        y_st = [ar.alloc([1024]) for _ in range(NY)]
        cg = [ar.alloc([NCT, 2]) for _ in range(2)]
        idx_i = [ar.alloc([NCT], I32) for _ in range(2)]
        b_xg = [Buf() for _ in range(NCT)]
        _bx = [Buf() for _ in range(NCT)]
        b_xgT = [_bx, _bx]
        b_act = [Buf() for _ in range(16)]
        b_sil = [Buf(), Buf()]
        b_y = [Buf() for _ in range(4)]
        b_cg = [Buf(), Buf()]
        b_idx = [Buf(), Buf()]
        b_p5 = [Buf() for _ in range(8)]
        cgsem = [p.new_dsem(), p.new_dsem()]
        gsem = [p.new_dsem() for _ in range(NCT)]
        scsem = [p.new_dsem() for _ in range(4)]

        def emit_gather(e_):
            sl = e_ % 2
            p.dma("sp", I_dma(cg[sl], comb_d[e_].rearrange("(ct p) t -> p ct t", p=128)), cgsem[sl],
                  writes=[b_cg[sl]], extra=[(csem[e_], csem[e_].total)])
            p.op("dve", I_copy(idx_i[sl], cg[sl][:, :, 0]), reads=[b_cg[sl]], writes=[b_idx[sl]])
            for ct in range(NCT):
                def ga(e, sl=sl, ct=ct):
                    return e.indirect_dma_start(
                        out=xg[:, ct, :], out_offset=None, in_=hs_d,
                        in_offset=bass.IndirectOffsetOnAxis(ap=idx_i[sl][:, ct:ct + 1], axis=0),
                        bounds_check=bc_reg(e, ROWS - 1), oob_is_err=True)
                p.dma("pool", ga, gsem[ct], reads=[b_idx[sl]], writes=[b_xg[ct]])

        p.op("dve", I_memset(actT, 0.0), writes=b_act)
        tr_cnt = [0]

        def emit_xg_transposes(e_):
            sl = e_ % 2
            for ct in range(NCT):
                bk = 6 + tr_cnt[0] % 2
                tr_cnt[0] += 1
                pb = bank(bk).bitcast(BF16).rearrange("p (c t) -> p c t", t=128)
                p.op("pe", I_trs([(pb[:, c, :], xg[:, ct, c * 128:(c + 1) * 128], ident_b) for c in range(8)]),
                     reads=[b_xg[ct], b_ident], writes=[b_p5[bk]])
                p.op("act" if ct % 2 == 0 else "dve",
                     (I_acopy if ct % 2 == 0 else I_copy)(xgT[sl][:, :, ct * 128:(ct + 1) * 128], pb),
                     reads=[b_p5[bk]], writes=[b_xgT[sl][ct]])

        CG = [(0, 342), (342, 342), (684, 342)]
        gu_cnt = [0]

        def emit_gateup(e_, fh):
            sl = e_ % 2
            XT = xgT[sl]
            for fc in range(8):
                f = fh * 8 + fc
                for gi, (c0, n) in enumerate(CG):
                    ab = gu_cnt[0] % 2
                    gu_cnt[0] += 1
                    pA = bank(2 * ab)
                    pB = bank(2 * ab + 1)
                    rx = [b_xgT[sl][ct] for ct in range(c0 // 128, (c0 + n + 127) // 128)]
                    p.op("pe", I_mm([(pA[:, 0:n], wslot[fh * 2][:, c, fc * 128:(fc + 1) * 128], XT[:, c, c0:c0 + n],
                                      c == 0, c == 7, {}) for c in range(8)]),
                         reads=rx + [b_ws[fh * 2]], writes=[b_p5[2 * ab]])
                    p.op("pe", I_mm([(pB[:, 0:n], wslot[fh * 2 + 1][:, c, fc * 128:(fc + 1) * 128],
                                      XT[:, c, c0:c0 + n], c == 0, c == 7, {}) for c in range(8)]),
                         reads=rx + [b_ws[fh * 2 + 1]], writes=[b_p5[2 * ab + 1]])
                    p.op("act", I_act(sil[ab][:, 0:n], pA[:, 0:n], AF.Silu), reads=[b_p5[2 * ab]], writes=[b_sil[ab]])
                    tok = p.op("dve", I_tt(actT[:, f, c0:c0 + n], sil[ab][:, 0:n], pB[:, 0:n], ALU.mult),
                               reads=[b_sil[ab], b_p5[2 * ab + 1]], extra=list(b_act[f].r) if gi == 0 else [])
                    if gi == 2:
                        b_act[f].w = [tok]
                        b_act[f].r = []

        y_cnt = [0]

        def emit_down(e_, ct):
            sl = e_ % 2
            ys = y_cnt[0] % NY
            y_cnt[0] += 1
            for half in range(2):
                pY = bank(4 + half)
                p.op("pe", I_mm([(pY, actT[:, f, ct * 128:(ct + 1) * 128],
                                  wslot[4 + f // 8][:, f % 8, half * 512:(half + 1) * 512], f == 0, f == 15, {})
                                 for f in range(16)]),
                     reads=b_act + [b_ws[4], b_ws[5]], writes=[b_p5[4 + half]])
                p.op("dve", I_ts(y_st[ys][:, half * 512:(half + 1) * 512], pY, cg[sl][:, ct, 1:2], None, ALU.mult),
                     reads=[b_p5[4 + half], b_cg[sl]], writes=[b_y[ys]] if half == 0 else [])
            b_y[ys].w = [("dve", p.cnt["dve"])]

            def sa(e, sl=sl, ct=ct, ys=ys):
                return e.indirect_dma_start(
                    out=hacc_d, out_offset=bass.IndirectOffsetOnAxis(ap=idx_i[sl][:, ct:ct + 1], axis=0),
                    in_=y_st[ys], in_offset=None, bounds_check=bc_reg(e, ROWS - 1), oob_is_err=True, compute_op=ALU.add)
            extra = list(prev_sc[0]) if ct == 0 else []
            p.dma("pool", sa, scsem[ys], reads=[b_y[ys], b_idx[sl]], extra=extra)
            if ct == NCT - 1:
                prev_sc[0] = [(sc_, sc_.total) for sc_ in scsem if sc_.total > 0]

        prev_sc = [[]]
        emit_gather(0)
        emit_comb_scatter(1)
        emit_xg_transposes(0)
        for e_ in range(NE):
            if e_ + 2 < NE and e_ > 0:
                emit_comb_scatter(e_ + 2, 0, 36)
            emit_gateup(e_, 0)
            if e_ + 1 < NE:
                load_w(e_ + 1, 0)
                load_w(e_ + 1, 1)
            if e_ + 2 < NE and e_ > 0:
                emit_comb_scatter(e_ + 2, 36, NT)
            if e_ + 1 < NE and e_ > 0:
                emit_gather(e_ + 1)
            emit_gateup(e_, 1)
            if e_ + 1 < NE:
                load_w(e_ + 1, 2)
                load_w(e_ + 1, 3)
                if e_ == 0:
                    emit_gather(e_ + 1)
            for ct in range(NCT):
                emit_down(e_, ct)
                if ct == 3 and e_ + 1 < NE:
                    emit_xg_transposes(e_ + 1)
            if e_ + 1 < NE:
                load_w(e_ + 1, 4)
                load_w(e_ + 1, 5)
            if e_ == 0:
                emit_comb_scatter(2)
        p.barrier()
        ar.top = persist_top

        ar.n = ARENA_WORDS
        g3b = ar.alloc([1024])
        hx = [ar.alloc([4, 1024]) for _ in range(3)]
        ox = [ar.alloc([4, 1024]) for _ in range(3)]
        jk6 = ar.alloc([1024], BF16)
        c6 = ar.alloc([16])
        b_g3 = Buf(); b_hx = [Buf() for _ in range(3)]; b_ox = [Buf() for _ in range(3)]; b_j6 = Buf(); b_s6 = Buf(); b_t6 = Buf(); b_r6 = Buf()
        l6 = [p.new_dsem() for _ in range(3)]
        s6 = [p.new_dsem() for _ in range(3)]
        p.load1(g3b, g3_d, b_g3)

        def load6(T):
            p.dma("sp", I_dma(hx[T % 3], hacc_d[T * 512:(T + 1) * 512, :].rearrange("(s p) d -> p s d", p=128)),
                  l6[T % 3], writes=[b_hx[T % 3]])

        load6(0)
        load6(1)
        for T in range(16):
            if T + 2 < 16:
                load6(T + 2)
            X = hx[T % 3]
            for s in range(4):
                p.op("act", I_act(jk6, X[:, s, :], AF.Square, accum_out=c6[:, s:s + 1]), reads=[b_hx[T % 3]],
                     writes=[b_j6, b_s6] if s == 0 else [b_j6])
            b_s6.w = [("act", p.cnt["act"])]
            rstd_chain(c6[:, 0:4], float(D), c6[:, 4:8], c6[:, 8:12], b_s6, b_t6, b_r6)
            for s in range(4):
                p.op("dve", I_stt(ox[T % 3][:, s, :], X[:, s, :], c6[:, 8 + s:9 + s], g3b, ALU.mult, ALU.mult),
                     reads=[b_hx[T % 3], b_r6, b_g3], writes=[b_ox[T % 3]] if s == 0 else [])
            b_ox[T % 3].w = [("dve", p.cnt["dve"])]
            p.dma("pool", I_dma(out_d[T * 512:(T + 1) * 512, :].rearrange("(s p) d -> p s d", p=128), ox[T % 3]),
                  s6[T % 3], reads=[b_ox[T % 3]])
        p.barrier()

        with nc.Block() as block:
            @block.sync
            def _(e):
                p.replay("sp", e)

            @block.gpsimd
            def _(e):
                p.replay("pool", e)

            @block.scalar
            def _(e):
                p.replay("act", e)

            @block.vector
            def _(e):
                p.replay("dve", e)

            @block.tensor
            def _(e):
                p.replay("pe", e)
    return nc


_NC_CACHE = {}


def _rope_table(scale):
    n = np.arange(LP)
    pos = np.where(n < SEQ, n + NMETA, n - SEQ)
    pos = np.where(n < L, pos, 0).astype(np.float32)
    inv_freq = (np.float32(10000.0) ** (-(np.arange(0, 64, 2, dtype=np.float32)) / np.float32(64))).astype(np.float32)
    ang = (pos[:, None] * inv_freq[None, :]).astype(np.float32)
    cos = np.cos(ang).astype(np.float32)
    sin = np.sin(ang).astype(np.float32)
    t = np.concatenate([cos, cos, -sin, sin], axis=1).astype(np.float32) * np.float32(scale)
    return np.ascontiguousarray(t)


def kernel(x, meta_tokens, mix_norm_g, w_in, conv_w, lambda_q1, lambda_k1, lambda_q2, lambda_k2, attn_subln_g,
           w_out, ffn_norm_g, w_router, w_gate, w_up, w_down, final_norm_g):
    f = lambda a: np.ascontiguousarray(np.asarray(a, dtype=np.float32))
    x = f(x)
    B = x.shape[0]
    if "nc" not in _NC_CACHE:
        _NC_CACHE["nc"] = build_nc()
    nc = _NC_CACHE["nc"]
    tile128 = lambda v: np.ascontiguousarray(np.broadcast_to(f(v).reshape(1, -1), (128, f(v).size)))
    convw = np.ascontiguousarray(f(conv_w)[0].reshape(3, 4, 128).transpose(2, 1, 0))
    lamp = np.ascontiguousarray(np.broadcast_to(
        np.stack([f(lambda_q1)[0], f(lambda_k1)[0], f(lambda_q2)[0], f(lambda_k2)[0]])[None], (128, 4, 64)))
    tokid = np.ascontiguousarray((np.arange(NT)[None, :] * 128 + np.arange(128)[:, None]).astype(np.float32))
    pp = np.arange(128)
    same = (pp[:, None] // 8) == (pp[None, :] // 8)
    gmat = np.ascontiguousarray(same.astype(np.float32))
    tmat = np.ascontiguousarray((same & (pp[:, None] < pp[None, :])).astype(np.float32))
    dummy = np.zeros((128, NCT, 2), np.float32)
    dummy[:, :, 0] = (L + np.arange(128))[:, None]
    shared = {
        "meta": f(meta_tokens),
        "g1b": tile128(f(mix_norm_g)[0]), "g2b": tile128(f(ffn_norm_g)[0]), "g3b": tile128(f(final_norm_g)),
        "w_in": f(w_in)[0], "w_out": f(w_out)[0], "w_router": f(w_router)[0],
        "w_gate": f(w_gate)[0], "w_up": f(w_up)[0], "w_down": f(w_down)[0],
        "convw": convw, "lamp": lamp, "gsub": tile128(f(attn_subln_g)[0]),
        "ropeq": _rope_table(0.125), "ropek": _rope_table(1.0),
        "ident": np.eye(128, dtype=np.float32), "tokid": tokid, "dummyrows": dummy,
        "gmat": gmat, "tmat": tmat,
    }
    in_maps = []
    for b in range(B):
        m = dict(shared)
        m["x"] = x[b]
        in_maps.append(m)
    res = run_bass_kernel_spmd(nc, in_maps, core_ids=list(range(B)))
    if DEBUG:
        _NC_CACHE["dbg"] = {k: np.asarray(res.results[0][k]) for k in DEBUG}
    return np.stack([np.asarray(r["out"], dtype=np.float32) for r in res.results], axis=0)
```
